# Optimizing a Trainium2 kernel written in Bass

```python
import jax, jax.numpy as jnp
from jax import lax
import numpy as np

D_MODEL = 1024
BATCH = 8
SEQ = 8192
DEPTH = 1

HEAD_DIM = 64
N_HEADS_DIL = 8
N_HEADS_SB = 8
D_DIL = N_HEADS_DIL * HEAD_DIM
D_SB = N_HEADS_SB * HEAD_DIM
D_MIX = D_DIL + D_SB
DILATION_PATTERNS = ((128, 1), (512, 4), (2048, 16))
SB_BLOCK = 128
N_GROUPS = 4
EXPERTS_PER_GROUP = 8
N_EXPERTS = N_GROUPS * EXPERTS_PER_GROUP
TOP_K = 2
D_FF_EXPERT = D_MODEL // 2
MOE_BLOCK = 128
NORM_EPS = 1e-6

kernel_name = "hymba_dilated_stickbreak_hmoe_block"


def _rmsnorm(x, g):
    xf = x.astype(jnp.float32)
    y = xf * lax.rsqrt(jnp.mean(xf * xf, axis=-1, keepdims=True) + NORM_EPS)
    return (y * g.astype(jnp.float32)).astype(x.dtype)


def _modulate(h, shift, scale):
    return h * (1 + scale[:, None, :]) + shift[:, None, :]


def _alibi_slopes(n_heads):
    return np.array([2.0 ** (-8.0 * (i + 1) / n_heads) for i in range(n_heads)], dtype=np.float32)


def _dilated_window_attention(q, k, v, window, dilation, slopes):
    b, s, h, e = q.shape
    n = window // dilation
    unit = n * dilation
    s_pad = -(-s // unit) * unit
    nb = s_pad // unit
    pad = ((0, 0), (0, s_pad - s), (0, 0), (0, 0))

    def split(t):
        return jnp.pad(t, pad).reshape(b, nb, n, dilation, h, e)

    def with_prev(t):
        prev = jnp.pad(t[:, :-1], ((0, 0), (1, 0), (0, 0), (0, 0), (0, 0), (0, 0)))
        return jnp.concatenate([prev, t], axis=2)

    qb = split(q)
    kc = with_prev(split(k))
    vc = with_prev(split(v)).astype(jnp.float32)
    scores = jnp.einsum('bgidhe,bgjdhe->bgdhij', qb, kc,
                        preferred_element_type=jnp.float32) * (e ** -0.5)
    i_idx = np.arange(n)[:, None]
    j_idx = np.arange(2 * n)[None, :]
    steps = i_idx + n - j_idx
    blk = np.arange(nb)[:, None, None]
    valid = (steps >= 0) & (steps <= n) & (blk * n + j_idx - n >= 0)
    bias = -slopes[:, None, None] * (steps * dilation).astype(np.float32)[None]
    logits = jnp.where(valid[None, :, None, None], scores + jnp.asarray(bias)[None, None, None], -jnp.inf)
    m = jnp.max(logits, axis=-1)
    p = jnp.exp(logits - m[..., None])
    den = jnp.sum(p, axis=-1)
    den_t = den.transpose(0, 1, 4, 2, 3)
    o = jnp.einsum('bgdhij,bgjdhe->bgidhe', p, vc) / den_t[..., None]
    o = o.reshape(b, s_pad, h, e)[:, :s]
    m = m.transpose(0, 1, 4, 2, 3).reshape(b, s_pad, h)[:, :s]
    den = den_t.reshape(b, s_pad, h)[:, :s]
    return o, m, den


def _dilated_mixture(q, k, v):
    slopes = _alibi_slopes(q.shape[2])
    outs, maxes, dens = [], [], []
    for window, dilation in DILATION_PATTERNS:
        o, m, den = _dilated_window_attention(q, k, v, window, dilation, slopes)
        outs.append(o)
        maxes.append(m)
        dens.append(den)
    outs = jnp.stack(outs)
    maxes = jnp.stack(maxes)
    dens = jnp.stack(dens)
    w = dens * jnp.exp(maxes - jnp.max(maxes, axis=0, keepdims=True))
    w = w / jnp.sum(w, axis=0, keepdims=True)
    return jnp.sum(w[..., None] * outs, axis=0).astype(q.dtype)


def _stick_breaking_attention(q, k, v):
    b, s, h, e = q.shape
    nb = s // SB_BLOCK
    qblocks = q.reshape(b, nb, SB_BLOCK, h, e).transpose(1, 0, 2, 3, 4)
    key_pos = jnp.arange(s)
    vf = v.astype(jnp.float32)

    def block(args):
        qb, idx = args
        z = jnp.einsum('bqhe,bshe->bhqs', qb, k, preferred_element_type=jnp.float32) * (e ** -0.5)
        q_pos = idx * SB_BLOCK + jnp.arange(SB_BLOCK)
        causal = key_pos[None, :] < q_pos[:, None]
        log_beta = jax.nn.log_sigmoid(z)
        log_keep = jnp.where(causal, jax.nn.log_sigmoid(-z), 0.0)
        between = lax.cumsum(log_keep, axis=3, reverse=True) - log_keep
        weights = jnp.where(causal, jnp.exp(log_beta + between), 0.0)
        return jnp.einsum('bhqs,bshe->bqhe', weights, vf)

    out = lax.map(block, (qblocks, jnp.arange(nb)))
    return out.transpose(1, 0, 2, 3, 4).reshape(b, s, h, e).astype(q.dtype)


def _hierarchical_moe(h, w_group, w_expert, w_gate, w_up, w_down):
    n, d = h.shape
    group_prob = jax.nn.softmax((h @ w_group).astype(jnp.float32), axis=-1)
    group = jnp.argmax(group_prob, axis=-1)
    group_gate = jnp.take_along_axis(group_prob, group[:, None], axis=1)
    exp_logits = (h @ w_expert).astype(jnp.float32).reshape(n, N_GROUPS, EXPERTS_PER_GROUP)
    exp_logits = jnp.take_along_axis(exp_logits, group[:, None, None], axis=1)[:, 0]
    top_p, top_local = lax.top_k(jax.nn.softmax(exp_logits, axis=-1), TOP_K)
    top_p = top_p / jnp.sum(top_p, axis=-1, keepdims=True)
    weight = (group_gate * top_p).reshape(-1)
    expert = (group[:, None] * EXPERTS_PER_GROUP + top_local).reshape(-1)
    token = jnp.repeat(jnp.arange(n), TOP_K)

    order = jnp.argsort(expert)
    e_sorted = expert[order]
    tok_sorted = token[order]
    w_sorted = weight[order]
    counts = jnp.zeros((N_EXPERTS,), jnp.int32).at[expert].add(1)
    padded = (counts + MOE_BLOCK - 1) // MOE_BLOCK * MOE_BLOCK
    start = jnp.cumsum(counts) - counts
    pad_end = jnp.cumsum(padded)
    pad_start = pad_end - padded
    nk = n * TOP_K
    dest = pad_start[e_sorted] + jnp.arange(nk) - start[e_sorted]
    cap = -(-nk // MOE_BLOCK) * MOE_BLOCK + N_EXPERTS * MOE_BLOCK
    n_blocks = cap // MOE_BLOCK
    buf = jnp.zeros((cap, d), h.dtype).at[dest].set(h[tok_sorted])
    block_expert = jnp.minimum(
        jnp.searchsorted(pad_end, jnp.arange(n_blocks) * MOE_BLOCK, side='right'), N_EXPERTS - 1)

    def expert_block(args):
        xb, e = args
        return (jax.nn.silu(xb @ w_gate[e]) * (xb @ w_up[e])) @ w_down[e]

    y = lax.map(expert_block, (buf.reshape(n_blocks, MOE_BLOCK, d), block_expert)).reshape(cap, d)
    contrib = y[dest] * w_sorted[:, None].astype(y.dtype)
    return jnp.zeros((n, d), y.dtype).at[tok_sorted].add(contrib)


def setup_inputs(seed: int = 0) -> dict:
    key = jax.random.key(seed)
    ks = jax.random.split(key, 16)
    d = D_MODEL

    def nrm(k, shape, std):
        return jax.random.normal(k, shape, jnp.float32) * std

    return {
        "x": nrm(ks[0], (BATCH, SEQ, d), 1.0),
        "c": nrm(ks[1], (BATCH, d), 1.0),
        "w_ada": nrm(ks[2], (DEPTH, d, 6 * d), 0.5 * d ** -0.5),
        "b_ada": nrm(ks[3], (DEPTH, 6 * d), 0.02),
        "g_mix": 1.0 + nrm(ks[4], (DEPTH, d), 0.02),
        "w_in": nrm(ks[5], (DEPTH, d, 3 * D_MIX), d ** -0.5),
        "g_dil_out": 1.0 + nrm(ks[6], (DEPTH, D_DIL), 0.02),
        "g_sb_out": 1.0 + nrm(ks[7], (DEPTH, D_SB), 0.02),
        "w_out": nrm(ks[8], (DEPTH, D_MIX, d), D_MIX ** -0.5),
        "g_ffn": 1.0 + nrm(ks[9], (DEPTH, d), 0.02),
        "w_group": nrm(ks[10], (DEPTH, d, N_GROUPS), d ** -0.5),
        "w_expert": nrm(ks[11], (DEPTH, d, N_EXPERTS), d ** -0.5),
        "w_gate": nrm(ks[12], (DEPTH, N_EXPERTS, d, D_FF_EXPERT), d ** -0.5),
        "w_up": nrm(ks[13], (DEPTH, N_EXPERTS, d, D_FF_EXPERT), d ** -0.5),
        "w_down": nrm(ks[14], (DEPTH, N_EXPERTS, D_FF_EXPERT, d), D_FF_EXPERT ** -0.5),
        "g_final": 1.0 + nrm(ks[15], (d,), 0.02),
    }


def reference(x, c, w_ada, b_ada, g_mix, w_in, g_dil_out, g_sb_out, w_out, g_ffn,
              w_group, w_expert, w_gate, w_up, w_down, g_final):
    b, s, d = x.shape
    cond = jax.nn.silu(c)
    for layer in range(DEPTH):
        mod = cond @ w_ada[layer] + b_ada[layer]
        shift_mix, scale_mix, gate_mix, shift_ffn, scale_ffn, gate_ffn = jnp.split(mod, 6, axis=-1)

        h = _modulate(_rmsnorm(x, g_mix[layer]), shift_mix, scale_mix)
        qkv = h @ w_in[layer]
        q_d, k_d, v_d, q_s, k_s, v_s = jnp.split(
            qkv, [D_DIL, 2 * D_DIL, 3 * D_DIL, 3 * D_DIL + D_SB, 3 * D_DIL + 2 * D_SB], axis=-1)
        heads_d = lambda t: t.reshape(b, s, N_HEADS_DIL, HEAD_DIM)
        heads_s = lambda t: t.reshape(b, s, N_HEADS_SB, HEAD_DIM)
        o_dil = _dilated_mixture(heads_d(q_d), heads_d(k_d), heads_d(v_d)).reshape(b, s, D_DIL)
        o_sb = _stick_breaking_attention(heads_s(q_s), heads_s(k_s), heads_s(v_s)).reshape(b, s, D_SB)
        mixed = jnp.concatenate([_rmsnorm(o_dil, g_dil_out[layer]),
                                 _rmsnorm(o_sb, g_sb_out[layer])], axis=-1)
        x = x + gate_mix[:, None, :] * (mixed @ w_out[layer])

        h2 = _modulate(_rmsnorm(x, g_ffn[layer]), shift_ffn, scale_ffn).reshape(b * s, d)
        y = _hierarchical_moe(h2, w_group[layer], w_expert[layer],
                              w_gate[layer], w_up[layer], w_down[layer]).reshape(b, s, d)
        x = x + gate_ffn[:, None, :] * y
    return _rmsnorm(x, g_final)
```

```python
import contextlib
import numpy as np
import ml_dtypes
import concourse.bass as bass
import concourse.mybir as mybir
from concourse.bass_utils import run_bass_kernel_spmd

F32 = mybir.dt.float32
BF16 = mybir.dt.bfloat16
I32 = mybir.dt.int32
AF = mybir.ActivationFunctionType
ALU = mybir.AluOpType
AX = mybir.AxisListType


class _Op:
    __slots__ = ("eng", "fn", "deps", "needed", "is_dma", "sem", "val", "pos")

    def __init__(self, eng, fn, is_dma):
        self.eng = eng
        self.fn = fn
        self.deps = set()
        self.needed = False
        self.is_dma = is_dma
        self.sem = None
        self.val = 0
        self.pos = 0


class Prog:
    ENGS = ("pe", "act", "dve", "pool", "sp")
    NSLOT = 12

    def __init__(self, nc, es):
        self.nc = nc
        self.streams = {e: [] for e in self.ENGS}
        self.last_writer = {}
        self.readers = {}
        self.esem = {e: es.enter_context(nc.semaphore("sem_" + e)) for e in self.ENGS}
        self.dsem = {
            q: [es.enter_context(nc.semaphore(f"dsem_{q}{i}")) for i in range(self.NSLOT)]
            for q in ("sp", "pool", "act")
        }
        self.ndma = {q: 0 for q in self.dsem}
        self.dma_last = {q: [None] * self.NSLOT for q in self.dsem}

    def _add(self, op, reads, writes):
        eng = op.eng
        for r in reads:
            w = self.last_writer.get(r)
            if w is not None:
                if w.is_dma or w.eng != eng or eng != "pe":
                    op.deps.add(w)
            if isinstance(r, tuple) and r and r[0] == "ps":
                rd = self.readers.get(r)
                if rd is not None:
                    for e2, o2 in rd[0].items():
                        if e2 != eng:
                            op.deps.add(o2)
        for r in writes:
            w = self.last_writer.get(r)
            if w is not None and (w.is_dma or w.eng != eng):
                op.deps.add(w)
            rd = self.readers.get(r)
            if rd is not None:
                for e2, o2 in rd[0].items():
                    if e2 != eng and o2 is not op:
                        op.deps.add(o2)
                for o2 in rd[1]:
                    if o2 is not op:
                        op.deps.add(o2)
        for r in reads:
            rd = self.readers.setdefault(r, ({}, []))
            if op.is_dma:
                rd[1].append(op)
            else:
                rd[0][eng] = op
        for r in writes:
            self.last_writer[r] = op
            self.readers[r] = ({}, [])
        op.deps.discard(op)
        self.streams[eng].append(op)
        return op

    def op(self, eng, fn, reads=(), writes=()):
        return self._add(_Op(eng, fn, False), reads, writes)

    def dma(self, q, out, in_, reads=(), writes=(), **kw):
        o = _Op(q, lambda e: e.dma_start(out=out, in_=in_, **kw), True)
        i = self.ndma[q]
        self.ndma[q] = i + 1
        o.sem = self.dsem[q][i % self.NSLOT]
        o.val = 16 * (i // self.NSLOT + 1)
        prev = self.dma_last[q][i % self.NSLOT]
        if prev is not None:
            o.deps.add(prev)
        self.dma_last[q][i % self.NSLOT] = o
        return self._add(o, reads, writes)

    def barrier(self):
        lasts = []
        for e in self.ENGS:
            for o in reversed(self.streams[e]):
                if not o.is_dma and o.fn is not None:
                    lasts.append(o)
                    break
        for q in self.dsem:
            for o in self.dma_last[q]:
                if o is not None:
                    lasts.append(o)
        for e in self.ENGS:
            o = _Op(e, None, False)
            o.deps = set(x for x in lasts if x.is_dma or x.eng != e)
            self.streams[e].append(o)

    def finish(self):
        self.barrier()
        for e in self.ENGS:
            for o in self.streams[e]:
                for d in o.deps:
                    d.needed = True
        for e in self.ENGS:
            c = 0
            for o in self.streams[e]:
                if o.is_dma or o.fn is None:
                    continue
                if o.needed:
                    c += 1
                    o.sem = self.esem[e]
                    o.val = c
        nc = self.nc
        with nc.Block() as block:
            for e, deco in (("pe", block.tensor), ("act", block.scalar), ("dve", block.vector),
                            ("pool", block.gpsimd), ("sp", block.sync)):
                stream = self.streams[e]

                def body(eng, stream=stream):
                    waited = {}
                    for o in stream:
                        need = {}
                        for d in o.deps:
                            k = id(d.sem)
                            if waited.get(k, 0) >= d.val:
                                continue
                            if k not in need or need[k][1] < d.val:
                                need[k] = (d.sem, d.val)
                        for k, (s, v) in need.items():
                            eng.wait_ge(s, v)
                            waited[k] = v
                        if o.fn is None:
                            continue
                        ins = o.fn(eng)
                        if o.is_dma:
                            ins.then_inc(o.sem, 16)
                        elif o.needed:
                            ins.then_inc(o.sem, 1)

                deco(body)


D = 1024
HD = 64
NH = 8
NEXP = 32
DFF = 512
EPS = 1e-6
PATTERNS = ((128, 1), (512, 4), (2048, 16))
MASKV = -240.0
NEG = -1.0e30


def _host_consts():
    f = np.float32
    c = {}
    c["ident"] = np.eye(128, dtype=f)
    j = np.arange(128)[:, None]
    s = np.arange(128)[None, :]
    c["negM"] = np.where(j >= s, -1.0, 0.0).astype(f)
    c["negN"] = np.where(j < s, -1.0, 0.0).astype(f)
    c["negI"] = (-np.eye(128)).astype(f)
    mb = np.zeros((128, 4, 512), f)
    for v in range(4):
        for cq in range(4):
            blk = np.zeros((128, 128), f)
            if cq < v:
                blk[:] = MASKV
            elif cq == v:
                blk = np.where(j < s, 0.0, MASKV).astype(f)
            mb[:, v, cq * 128:(cq + 1) * 128] = blk
    c["mb"] = mb.reshape(128, 2048)
    slopes = np.array([2.0 ** (-8.0 * (i + 1) / NH) for i in range(NH)], dtype=f)
    db = np.zeros((128, NH, 3, 256), f)
    for h in range(NH):
        for p, (w, d) in enumerate(PATTERNS):
            i = np.arange(128)[None, :]
            jj = np.arange(128)[:, None]
            cur = np.where(jj <= i, -slopes[h] * ((i - jj) * d).astype(f), NEG)
            prev = np.where(jj >= i, -slopes[h] * ((i + 128 - jj) * d).astype(f), NEG)
            db[:, h, p, 0:128] = cur
            db[:, h, p, 128:256] = prev
    c["dbias"] = db.reshape(128, NH * 3 * 256)
    sh = np.zeros((128, 64), f)
    sh[64 + np.arange(64), np.arange(64)] = 1.0
    c["shiftm"] = sh
    return c


def build_nc(S, phases=(0, 1, 2, 3, 4, 5), dbg=False):
    NT = S // 128
    NG = S // 512
    nc = bass.Bass("TRN2", target_bir_lowering=False)

    def din(name, shape, dt=F32):
        return nc.dram_tensor(name, list(shape), dt, kind="ExternalInput").ap()

    def dscr(name, shape, dt):
        return nc.dram_tensor(name, list(shape), dt, kind=("ExternalOutput" if dbg else "Internal")).ap()

    x = din("x", [S, D])
    cvec = din("c", [1, D])
    w_ada = din("w_ada", [D, 6 * D])
    b_ada = din("b_ada", [1, 6 * D])
    g_mix = din("g_mix", [1, D])
    w_in = din("w_in", [D, 3 * D])
    g_dil = din("g_dil_out", [1, 512])
    g_sb = din("g_sb_out", [1, 512])
    w_out = din("w_out", [D, D])
    g_ffn = din("g_ffn", [1, D])
    w_rt = din("w_rt", [D, 36])
    w_gate = din("w_gate", [NEXP, D, DFF])
    w_up = din("w_up", [NEXP, D, DFF])
    w_down = din("w_down", [NEXP, DFF, D])
    g_fin = din("g_final", [1, D])
    k_ident = din("k_ident", [128, 128])
    k_negM = din("k_negM", [128, 128])
    k_negN = din("k_negN", [128, 128])
    k_negI = din("k_negI", [128, 128])
    k_mb = din("k_mb", [128, 2048])
    k_dbias = din("k_dbias", [128, NH * 3 * 256])
    k_shiftm = din("k_shiftm", [128, 64])
    out = nc.dram_tensor("out", [S, D], F32, kind="ExternalOutput").ap()

    qT = dscr("qT", [D, S], BF16)
    kT = dscr("kT", [D, S], BF16)
    vv = dscr("vv", [S, D], BF16)
    attT = dscr("attT", [D, S], F32)
    x1 = dscr("x1", [S, D], F32)
    h2T = dscr("h2T", [D, S], BF16)

    with contextlib.ExitStack() as es:
        def sb(name, shape, dt, stack=es):
            return stack.enter_context(nc.sbuf_tensor(name, list(shape), dt))

        P = Prog(nc, es)
        psum = es.enter_context(nc.psum_tensor("psum", [128, 4096], F32))

        def bank(b, lo=0, hi=512, p0=0, p1=128):
            return psum[p0:p1, b * 512 + lo:b * 512 + hi]

        def PS(b):
            return ("ps", b)

        ident = sb("ident", [128, 128], F32)
        identb = sb("identb", [128, 128], BF16)
        negIb = sb("negIb", [128, 128], BF16)
        negMb = sb("negMb", [128, 128], BF16)
        negNb = sb("negNb", [128, 128], BF16)
        mbb = sb("mbb", [128, 2048], BF16)
        shiftm = sb("shiftm", [128, 64], F32)
        ones_f = sb("ones_f", [128, 128], F32)
        ones_b = sb("ones_b", [128, 64], BF16)
        vecT = sb("vecT", [128, 32], F32)
        AB = sb("AB", [128, 32], F32)
        gmix_bc = sb("gmix_bc", [128, D], F32)
        gffn_bc = sb("gffn_bc", [128, D], F32)
        gfin_bc = sb("gfin_bc", [128, D], F32)
        Wr = sb("Wr", [128, NT * 32], F32)

        stg = [sb(f"stg{i}", [128, 2048], F32) for i in range(4)]
        nstg = {"act": 0}

        def load_cast(dst, src, n, eng, dtoks, view=None, rd=()):
            if eng == "act":
                i = nstg["act"] % 2
                nstg["act"] += 1
            else:
                i = 2 if eng == "pool" else 3
            sv = stg[i][:, 0:n]
            if view is not None:
                sv = sv.rearrange(view[0], **view[1])
            P.dma("sp", sv, src, reads=list(rd), writes=[("stg", i)])
            if eng == "act":
                P.op("act", lambda e: e.activation(dst, sv, AF.Copy), reads=[("stg", i)], writes=list(dtoks))
            else:
                P.op(eng, lambda e: e.tensor_copy(dst, sv), reads=[("stg", i)], writes=list(dtoks))

        P.dma("sp", ident[:], k_ident, writes=["ident"])
        P.dma("sp", shiftm[:], k_shiftm, writes=["shiftm"])
        load_cast(identb[:], k_ident, 128, "dve", ["identb"])
        load_cast(negIb[:], k_negI, 128, "dve", ["negIb"])
        load_cast(negMb[:], k_negM, 128, "dve", ["negMb"])
        load_cast(negNb[:], k_negN, 128, "dve", ["negNb"])
        load_cast(mbb[:], k_mb, 2048, "dve", ["mbb"])
        P.op("dve", lambda e: e.memset(ones_f[:], 1.0), writes=["ones_f"])
        P.op("dve", lambda e: e.memset(ones_b[:], 1.0), writes=["ones_b"])

        def phase_0():
            with contextlib.ExitStack() as ps0:
                vecs = sb("vecs", [32, 128], F32, ps0)
                sc = sb("sc", [128, 8], F32, ps0)
                wa = [sb(f"wa{i}", [128, 8, 512], F32, ps0) for i in range(2)]
                brow = sb("brow", [1, 6 * D], F32, ps0)
                mrow = sb("mrow", [1, 6 * D], F32, ps0)
                gfrow = sb("gfrow", [1, D], F32, ps0)
                for r0, src, n in ((0, cvec, 8), (8, g_mix, 8), (16, g_ffn, 8), (24, g_dil, 4), (28, g_sb, 4)):
                    P.dma("sp", vecs[r0:r0 + n, :], src.rearrange("o (k p) -> (o k) p", p=128),
                          writes=[("vecs", r0)])
                P.dma("sp", brow[:], b_ada, writes=["brow"])
                P.dma("sp", gfrow[:], g_fin, writes=["gfrow"])
                P.op("pe", lambda e: e.transpose(bank(0, 0, 32), vecs[:, :], ident[0:32, 0:32]),
                     reads=[("vecs", r) for r in (0, 8, 16, 24, 28)] + ["ident"], writes=[PS(0)])
                P.op("dve", lambda e: e.tensor_copy(vecT[:], bank(0, 0, 32)), reads=[PS(0)], writes=["vecT"])
                P.op("act", lambda e: e.activation(sc[:], vecT[:, 0:8], AF.Silu), reads=["vecT"], writes=["sc"])
                for blk in range(12):
                    b = blk % 2
                    P.dma("sp", wa[b][:], w_ada[:, blk * 512:(blk + 1) * 512].rearrange("(k p) n -> p k n", p=128),
                          writes=[("wa", b)])
                    pb = 1 + b
                    for k in range(8):
                        P.op("pe", lambda e, k=k, b=b, pb=pb: e.matmul(
                            bank(pb, 0, 512, 0, 1), sc[:, k:k + 1], wa[b][:, k, :], start=(k == 0), stop=(k == 7)),
                            reads=["sc", ("wa", b)], writes=[PS(pb)])
                    P.op("dve", lambda e, blk=blk, pb=pb: e.tensor_tensor(
                        mrow[0:1, blk * 512:(blk + 1) * 512], bank(pb, 0, 512, 0, 1),
                        brow[0:1, blk * 512:(blk + 1) * 512], ALU.add),
                        reads=[PS(pb), "brow"], writes=[("mrow", blk)])
                mrow_all = [("mrow", i) for i in range(12)]
                for si, seg in enumerate((0, 1, 3, 4)):
                    for k in range(8):
                        col = si * 8 + k
                        P.op("pe", lambda e, seg=seg, k=k, col=col: e.matmul(
                            bank(3, col, col + 1), mrow[0:1, seg * D + k * 128: seg * D + (k + 1) * 128],
                            ones_f[0:1, 0:1], start=True, stop=True),
                            reads=mrow_all + ["ones_f"], writes=[PS(3)])
                P.op("dve", lambda e: e.scalar_tensor_tensor(AB[:, 0:8], bank(3, 8, 16), 1.0, vecT[:, 8:16],
                                                             ALU.add, ALU.mult),
                     reads=[PS(3), "vecT"], writes=[("AB", 0)])
                P.op("dve", lambda e: e.tensor_copy(AB[:, 8:16], bank(3, 0, 8)), reads=[PS(3)], writes=[("AB", 1)])
                P.op("dve", lambda e: e.scalar_tensor_tensor(AB[:, 16:24], bank(3, 24, 32), 1.0, vecT[:, 16:24],
                                                             ALU.add, ALU.mult),
                     reads=[PS(3), "vecT"], writes=[("AB", 2)])
                P.op("dve", lambda e: e.tensor_copy(AB[:, 24:32], bank(3, 16, 24)), reads=[PS(3)], writes=[("AB", 3)])
                for (dst, nm, row, off) in ((gmix_bc, "gmix_bc", mrow, 2 * D), (gffn_bc, "gffn_bc", mrow, 5 * D),
                                            (gfin_bc, "gfin_bc", gfrow, 0)):
                    for half in range(2):
                        pb = 4 + half
                        P.op("pe", lambda e, row=row, off=off, half=half, pb=pb: e.matmul(
                            bank(pb), ones_f[0:1, 0:128], row[0:1, off + half * 512: off + (half + 1) * 512],
                            start=True, stop=True),
                            reads=mrow_all + ["gfrow", "ones_f"], writes=[PS(pb)])
                        P.op("act", lambda e, dst=dst, half=half, pb=pb: e.activation(
                            dst[:, half * 512:(half + 1) * 512], bank(pb), AF.Copy),
                            reads=[PS(pb)], writes=[(nm, half)])
                P.barrier()

        if 0 in phases:
            phase_0()

        AB_all = [("AB", i) for i in range(4)]

        def norm_stats(xt, xt_toks, tag, ss, lnv, rstd, junk, y, y_tok):
            P.op("act", lambda e: e.activation(junk[:], xt[:], AF.Square, accum_out=ss[:]),
                 reads=list(xt_toks), writes=[(tag, "junk"), (tag, "ss")])
            P.op("act", lambda e: e.activation(lnv[:], ss[:], AF.Ln, bias=EPS, scale=1.0 / D),
                 reads=[(tag, "ss")], writes=[(tag, "lnv")])
            P.op("act", lambda e: e.activation(rstd[:], lnv[:], AF.Exp, scale=-0.5),
                 reads=[(tag, "lnv")], writes=[(tag, "rstd")])
            P.op("dve", lambda e: e.tensor_scalar(y[:], xt[:], rstd[:, 0:1], None, ALU.mult),
                 reads=list(xt_toks) + [(tag, "rstd")], writes=[y_tok])

        def norm_transp(Acol, dsts, dst_toks, pbanks, y, y_tok, act_only=False):
            for j in range(8):
                pb = pbanks[j // 4]
                P.op("pe", lambda e, j=j, pb=pb: e.transpose(
                    bank(pb, (j % 4) * 128, (j % 4 + 1) * 128), y[:, j * 128:(j + 1) * 128], ident[:]),
                    reads=[y_tok, "ident"], writes=[PS(pb)])
            for j in range(8):
                pb = pbanks[j // 4]
                src = bank(pb, (j % 4) * 128, (j % 4 + 1) * 128)
                for di, (dst_fn, dtok) in enumerate(zip(dsts, dst_toks)):
                    use_act = act_only or ((j // 4 + di) % 2 == 0)
                    if use_act:
                        P.op("act", lambda e, j=j, src=src, dst_fn=dst_fn: e.activation(
                            dst_fn(j), src, AF.Identity, bias=AB[:, Acol + 8 + j:Acol + 9 + j],
                            scale=AB[:, Acol + j:Acol + j + 1]),
                            reads=[PS(pb)] + AB_all, writes=[dtok])
                    else:
                        P.op("dve", lambda e, j=j, src=src, dst_fn=dst_fn: e.tensor_scalar(
                            dst_fn(j), src, AB[:, Acol + j:Acol + j + 1], AB[:, Acol + 8 + j:Acol + 9 + j],
                            ALU.mult, ALU.add),
                            reads=[PS(pb)] + AB_all, writes=[dtok])

        def phase_1():
            with contextlib.ExitStack() as ps1:
                win = sb("win", [128, 8, 3 * D], BF16, ps1)
                xb = [sb(f"xb{i}", [128, D], F32, ps1) for i in range(2)]
                yb = [sb(f"yb{i}", [128, D], F32, ps1) for i in range(2)]
                junk = sb("junk1", [128, D], BF16, ps1)
                st = [[sb(f"st{n}{i}", [128, 1], F32, ps1) for i in range(2)] for n in ("ss", "ln", "rs")]
                hT = [sb(f"hT{i}", [128, 8, 512], BF16, ps1) for i in range(2)]
                qst = [sb(f"qst{i}", [128, 512], BF16, ps1) for i in range(4)]
                vst = [sb(f"vst{i}", [128, D], BF16, ps1) for i in range(2)]
                for k in range(8):
                    for half in range(2):
                        load_cast(win[:, k, half * 1536:(half + 1) * 1536],
                                  w_in[k * 128:(k + 1) * 128, half * 1536:(half + 1) * 1536], 1536,
                                  ("pool", "dve")[half], [("win", k, half)])
                win_all = [("win", k, half) for k in range(8) for half in range(2)]
                qk = []
                for i in range(4):
                    qk.append((0 + 128 * i, qT, 128 * i, 0.125))
                    qk.append((512 + 128 * i, kT, 128 * i, 1.0))
                    qk.append((1536 + 128 * i, qT, 512 + 128 * i, 0.125))
                    qk.append((2048 + 128 * i, kT, 512 + 128 * i, 1.0))
                nev = 0
                for g in range(NG):
                    hb = g % 2
                    for t0_ in (0, 2):
                        for tt in (t0_, t0_ + 1):
                            t = g * 4 + tt
                            b = t % 2
                            P.dma("sp", xb[b][:], x[t * 128:(t + 1) * 128, :], writes=[("xb", b)])
                            norm_stats(xb[b], [("xb", b)], ("n1", b), st[0][b], st[1][b], st[2][b], junk, yb[b], ("yb", b))
                        for tt in (t0_, t0_ + 1):
                            t = g * 4 + tt
                            b = t % 2
                            norm_transp(0, [lambda j, hb=hb, tt=tt: hT[hb][:, j, tt * 128:(tt + 1) * 128]],
                                        [("hT", hb, tt)], (2 * b, 2 * b + 1), yb[b], ("yb", b))
                    hT_all = [("hT", hb, tt) for tt in range(4)]
                    for ci, (col, dst, row, scl) in enumerate(qk):
                        pb = 4 + (nev % 4)
                        for k in range(8):
                            P.op("pe", lambda e, k=k, col=col, hb=hb, pb=pb: e.matmul(
                                bank(pb), win[:, k, col:col + 128], hT[hb][:, k, :], start=(k == 0), stop=(k == 7)),
                                reads=win_all + hT_all, writes=[PS(pb)])
                        sbi = nev % 4
                        if nev % 2 == 0:
                            P.op("act", lambda e, sbi=sbi, pb=pb, scl=scl: e.activation(
                                qst[sbi][:], bank(pb), AF.Copy, scale=scl), reads=[PS(pb)], writes=[("qst", sbi)])
                        else:
                            P.op("dve", lambda e, sbi=sbi, pb=pb, scl=scl: e.tensor_scalar(
                                qst[sbi][:], bank(pb), scl, None, ALU.mult), reads=[PS(pb)], writes=[("qst", sbi)])
                        P.dma("sp", dst[row:row + 128, g * 512:(g + 1) * 512], qst[sbi][:],
                              reads=[("qst", sbi)], writes=[("qk", id(dst), row, g)])
                        nev += 1
                    for tt in range(4):
                        t = g * 4 + tt
                        vb = t % 2
                        for half, col in enumerate((1024, 2560)):
                            pb = 4 + (nev % 4)
                            for k in range(8):
                                P.op("pe", lambda e, k=k, col=col, hb=hb, tt=tt, pb=pb: e.matmul(
                                    bank(pb), hT[hb][:, k, tt * 128:(tt + 1) * 128], win[:, k, col:col + 512],
                                    start=(k == 0), stop=(k == 7)),
                                    reads=win_all + hT_all, writes=[PS(pb)])
                            if nev % 2 == 0:
                                P.op("act", lambda e, vb=vb, half=half, pb=pb: e.activation(
                                    vst[vb][:, half * 512:(half + 1) * 512], bank(pb), AF.Copy),
                                    reads=[PS(pb)], writes=[("vst", vb, half)])
                            else:
                                P.op("dve", lambda e, vb=vb, half=half, pb=pb: e.tensor_copy(
                                    vst[vb][:, half * 512:(half + 1) * 512], bank(pb)),
                                    reads=[PS(pb)], writes=[("vst", vb, half)])
                            nev += 1
                        P.dma("sp", vv[t * 128:(t + 1) * 128, :], vst[vb][:],
                              reads=[("vst", vb, 0), ("vst", vb, 1)], writes=[("vv", t)])
                P.barrier()

        if 1 in phases:
            phase_1()

        def phase_2():
            with contextlib.ExitStack() as ps2:
                qh = [sb(f"dqh{i}", [64, S], BF16, ps2) for i in range(2)]
                kh = [sb(f"dkh{i}", [64, S], BF16, ps2) for i in range(2)]
                vp1 = [sb(f"dvp_{p}", [128, NT, 128], BF16, ps2) for p in range(3)]
                vp = [vp1, vp1]
                for p_ in range(3):
                    P.op("dve", lambda e, p_=p_: e.memset(vp1[p_][:, :, 64:128], 1.0), writes=[("dvp1", p_)])
                dbs = [sb(f"dbs{i}", [128, 3 * 256], F32, ps2) for i in range(2)]
                ssb = [sb(f"dss{i}", [128, 256], F32, ps2) for i in range(4)]
                wsb = [sb(f"dws{i}", [128, 256], BF16, ps2) for i in range(4)]
                accs = sb("daccs", [128, 2048], F32, ps2)
                lnd = [sb(f"dlnd{i}", [64, 512], F32, ps2) for i in range(2)]
                rdn = [sb(f"drdn{i}", [64, 512], F32, ps2) for i in range(2)]
                ost = [sb(f"dost{i}", [64, 512], F32, ps2) for i in range(2)]
                nsc = 0
                for h in range(NH):
                    hb = h % 2
                    P.dma("sp", qh[hb][:], qT[h * 64:(h + 1) * 64, :],
                          reads=[("qk", id(qT), (h // 2) * 128, g) for g in range(NG)], writes=[("dqh", hb)])
                    P.dma("sp", kh[hb][:], kT[h * 64:(h + 1) * 64, :],
                          reads=[("qk", id(kT), (h // 2) * 128, g) for g in range(NG)], writes=[("dkh", hb)])
                    P.dma("sp", dbs[hb][:], k_dbias[:, h * 768:(h + 1) * 768], writes=[("dbs", hb)])
                    for p, (w_, d) in enumerate(PATTERNS):
                        nb = S // (128 * d)
                        if d == 1:
                            P.dma("sp", vp[hb][p][:, :, 0:64], vv[:, h * 64:(h + 1) * 64].rearrange("(t j) e -> j t e", j=128),
                                  reads=[("vv", t) for t in range(NT)], writes=[("dvp", 0, p, 0)])
                        else:
                            for b_ in range(nb):
                                P.dma("sp", vp[hb][p][:, b_ * d:(b_ + 1) * d, 0:64],
                                      vv[b_ * 128 * d:(b_ + 1) * 128 * d, h * 64:(h + 1) * 64].rearrange(
                                          "(j r) e -> j r e", r=d),
                                      reads=[("vv", t) for t in range(NT)], writes=[("dvp", 0, p, b_)])
                    for sbk in range(S // 2048):
                        first_in_bank = [True] * 4
                        combos = []
                        for p, (w_, d) in enumerate(PATTERNS):
                            for cb in range(16):
                                if d == 1:
                                    blk, r = sbk * 16 + cb, 0
                                elif d == 4:
                                    blk, r = sbk * 4 + cb // 4, cb % 4
                                else:
                                    blk, r = sbk, cb
                                combos.append((p, d, cb, blk, r))

                        def stage_a(p, d, cb, blk, r, si, pb, hb=hb):
                            has_prev = blk > 0
                            wdt = 256 if has_prev else 128
                            c0 = blk * 128 * d + r
                            cur = slice(c0, c0 + 127 * d + 1, d)
                            prv = slice(c0 - 128 * d, c0 - d + 1, d)
                            rd = [("dqh", hb), ("dkh", hb)]
                            P.op("pe", lambda e: e.matmul(
                                bank(pb, 0, 128), kh[hb][:, cur], qh[hb][:, cur], start=True, stop=True),
                                reads=rd, writes=[PS(pb)])
                            if has_prev:
                                P.op("pe", lambda e: e.matmul(
                                    bank(pb, 128, 256), kh[hb][:, prv], qh[hb][:, cur], start=True, stop=True),
                                    reads=rd, writes=[PS(pb)])
                            P.op("dve", lambda e: e.tensor_tensor(
                                ssb[si][:, 0:wdt], bank(pb, 0, wdt), dbs[hb][:, p * 256:p * 256 + wdt], ALU.add),
                                reads=[PS(pb), ("dbs", hb)], writes=[("dss", si)])
                            P.op("act", lambda e: e.activation(wsb[si][:, 0:wdt], ssb[si][:, 0:wdt], AF.Exp),
                                 reads=[("dss", si)], writes=[("dws", si)])

                        def stage_b(p, d, cb, blk, r, si, pb, hb=hb, first_in_bank=first_in_bank):
                            has_prev = blk > 0
                            combo = blk * d + r
                            pcombo = (blk - 1) * d + r
                            for half in range(2 if has_prev else 1):
                                vc = combo if half == 0 else pcombo
                                npc = 4 if d == 16 else 1
                                for pc in range(npc):
                                    if d == 16:
                                        bk = pc
                                        qs = slice(half * 128 + pc * 32, half * 128 + pc * 32 + 32)
                                        ocols = slice(bk * 512 + r, bk * 512 + r + 31 * 16 + 1, 16)
                                    elif d == 4:
                                        bk = cb // 4
                                        qs = slice(half * 128, half * 128 + 128)
                                        ocols = slice(bk * 512 + r, bk * 512 + r + 127 * 4 + 1, 4)
                                    else:
                                        bk = cb // 4
                                        qs = slice(half * 128, half * 128 + 128)
                                        lo = (cb % 4) * 128
                                        ocols = slice(bk * 512 + lo, bk * 512 + lo + 128)
                                    st_ = first_in_bank[bk]
                                    first_in_bank[bk] = False
                                    P.op("pe", lambda e, vc=vc, qs=qs, ocols=ocols, st_=st_: e.matmul(
                                        psum[0:128, ocols], vp[hb][p][:, vc, :], wsb[si][:, qs],
                                        start=st_, stop=True, skip_group_check=True),
                                        reads=[("dvp", 0, p, (vc // d if d > 1 else 0)), ("dvp1", p), ("dws", si)],
                                        writes=[PS(bk)])

                        LOOK = 2
                        slots = []
                        for ci, cmb in enumerate(combos):
                            slots.append((nsc % 4, 4 + nsc % 4))
                            nsc += 1
                        for ci in range(len(combos) + LOOK):
                            if ci < len(combos):
                                stage_a(*combos[ci], *slots[ci])
                            if ci >= LOOK:
                                stage_b(*combos[ci - LOOK], *slots[ci - LOOK])
                        for bk in range(4):
                            if bk % 2 == 0:
                                P.op("act", lambda e, bk=bk: e.activation(
                                    accs[:, bk * 512:(bk + 1) * 512], bank(bk), AF.Copy),
                                    reads=[PS(bk)], writes=[("daccs", bk)])
                            else:
                                P.op("dve", lambda e, bk=bk: e.tensor_copy(
                                    accs[:, bk * 512:(bk + 1) * 512], bank(bk)),
                                    reads=[PS(bk)], writes=[("daccs", bk)])
                        for bk in range(4):
                            pb = 4 + nsc % 4
                            nsc += 1
                            i2 = bk % 2
                            P.op("pe", lambda e, bk=bk, pb=pb: e.matmul(
                                bank(pb, 0, 512, 0, 64), shiftm[:, :], accs[:, bk * 512:(bk + 1) * 512],
                                start=True, stop=True),
                                reads=["shiftm", ("daccs", bk)], writes=[PS(pb)])
                            P.op("act", lambda e, pb=pb, i2=i2: e.activation(
                                lnd[i2][:], bank(pb, 0, 512, 0, 64), AF.Ln),
                                reads=[PS(pb)], writes=[("dlnd", i2)])
                            P.op("act", lambda e, i2=i2: e.activation(rdn[i2][:], lnd[i2][:], AF.Exp, scale=-1.0),
                                 reads=[("dlnd", i2)], writes=[("drdn", i2)])
                            P.op("dve", lambda e, bk=bk, i2=i2: e.tensor_tensor(
                                ost[i2][:], accs[0:64, bk * 512:(bk + 1) * 512], rdn[i2][:], ALU.mult),
                                reads=[("daccs", bk), ("drdn", i2)], writes=[("dost", i2)])
                            c0 = sbk * 2048 + bk * 512
                            P.dma("sp", attT[h * 64:(h + 1) * 64, c0:c0 + 512], ost[i2][:],
                                  reads=[("dost", i2)], writes=[("attT", h, c0 // 512)])
                P.barrier()

        if 2 in phases:
            phase_2()

        def phase_3():
            with contextlib.ExitStack() as ps3:
                qp = [sb(f"sqp{i}", [128, S], BF16, ps3) for i in range(2)]
                kp = [sb(f"skp{i}", [128, S], BF16, ps3) for i in range(2)]
                vp = [sb(f"svp{i}", [128, NT, 128], BF16, ps3) for i in range(2)]
                eb = [sb(f"seb{i}", [128, 1024], F32, ps3) for i in range(3)]
                spb = [sb(f"ssp{i}", [128, 1024], BF16, ps3) for i in range(2)]
                ub = [sb(f"sub{i}", [128, 1024], F32, ps3) for i in range(2)]
                wb = [sb(f"swb{i}", [128, 1024], BF16, ps3) for i in range(2)]
                osb = [sb(f"sos{i}", [128, 512], F32, ps3) for i in range(2)]
                OB = 6

                def sb_pair(hp, pb_):
                    row = 512 + hp * 128
                    P.dma("sp", qp[pb_][:], qT[row:row + 128, :],
                          reads=[("qk", id(qT), row, g) for g in range(NG)], writes=[("sqp", pb_)])
                    P.dma("sp", kp[pb_][:], kT[row:row + 128, :],
                          reads=[("qk", id(kT), row, g) for g in range(NG)], writes=[("skp", pb_)])
                    P.dma("sp", vp[pb_][:], vv[:, row:row + 128].rearrange("(t p) e -> p t e", p=128),
                          reads=[("vv", t) for t in range(NT)], writes=[("svp", pb_)])
                    rdqk = [("sqp", pb_), ("skp", pb_)]

                    def emit_z(g, kb, i):
                        v = kb - 4 * g
                        diag = v >= 0
                        qs = slice(g * 512, (g + 1) * 512)
                        ks = slice(kb * 128, (kb + 1) * 128)
                        for s_ in range(2):
                            zb = 2 * (i % 2) + s_
                            ps_ = slice(64 * s_, 64 * s_ + 64)
                            P.op("pe", lambda e, zb=zb, ps_=ps_: e.matmul(
                                bank(zb), kp[pb_][ps_, ks], qp[pb_][ps_, qs], start=True, stop=not diag),
                                reads=rdqk, writes=[PS(zb)])
                        if diag:
                            for s_ in range(2):
                                zb = 2 * (i % 2) + s_
                                P.op("pe", lambda e, zb=zb: e.matmul(
                                    bank(zb), identb[:], mbb[:, v * 512:(v + 1) * 512], start=False, stop=True),
                                    reads=["identb", "mbb"], writes=[PS(zb)])

                    hs2 = lambda s_: slice(512 * s_, 512 * s_ + 512)

                    def emit_e(i):
                        j, j3 = i % 2, i % 3
                        P.op("act", lambda e: e.activation(eb[j3][:], psum[:, 2 * j * 512:(2 * j + 2) * 512], AF.Exp),
                             reads=[PS(2 * j), PS(2 * j + 1)], writes=[("seb", j3)])

                    def emit_sp(i):
                        j, j3 = i % 2, i % 3
                        P.op("act", lambda e: e.activation(spb[j][:], eb[j3][:], AF.Ln, bias=1.0),
                             reads=[("seb", j3)], writes=[("ssp", j)])

                    def emit_negM(i):
                        j = i % 2
                        for s_ in range(2):
                            P.op("pe", lambda e, s_=s_: e.matmul(bank(4 + s_), negMb[:], spb[j][:, hs2(s_)],
                                                                 start=(i == 0), stop=True, skip_group_check=True),
                                 reads=["negMb", ("ssp", j)], writes=[PS(4 + s_)])

                    def emit_u(i):
                        j = i % 2
                        P.op("act", lambda e: e.activation(ub[j][:], psum[:, 4 * 512:6 * 512], AF.Exp),
                             reads=[PS(4), PS(5)], writes=[("sub", j)])

                    def emit_w(i):
                        j, j3 = i % 2, i % 3
                        P.op("dve", lambda e: e.tensor_tensor(wb[j][:], eb[j3][:], ub[j][:], ALU.mult),
                             reads=[("seb", j3), ("sub", j)], writes=[("swb", j)])

                    def emit_negN(i):
                        j = i % 2
                        for s_ in range(2):
                            P.op("pe", lambda e, s_=s_: e.matmul(bank(4 + s_), negNb[:], spb[j][:, hs2(s_)],
                                                                 start=False, stop=True, skip_group_check=True),
                                 reads=["negNb", ("ssp", j), ("sub", j)], writes=[PS(4 + s_)])

                    def emit_pv(kb, i, n):
                        j = i % 2
                        for s_ in range(2):
                            P.op("pe", lambda e, s_=s_: e.matmul(
                                bank(OB, 0, 512, 64 * s_, 64 * s_ + 64), vp[pb_][:, kb, 64 * s_:64 * s_ + 64],
                                wb[j][:, hs2(s_)], start=(i == 0), stop=(i == n - 1), skip_group_check=True),
                                reads=[("svp", pb_), ("swb", j)], writes=[PS(OB)])

                    for g in range(NG):
                        kbs = list(range(4 * g + 3, -1, -1))
                        n = len(kbs)
                        emit_z(g, kbs[0], 0)
                        emit_z(g, kbs[1], 1)
                        emit_e(0)
                        emit_z(g, kbs[2], 2)
                        emit_e(1)
                        emit_sp(0)
                        emit_negM(0)
                        for i in range(n):
                            if i + 1 < n:
                                emit_sp(i + 1)
                            emit_u(i)
                            if i + 2 < n:
                                emit_e(i + 2)
                            emit_w(i)
                            if i + 1 < n:
                                emit_negN(i)
                                emit_negM(i + 1)
                            emit_pv(kbs[i], i, n)
                            if i + 3 < n:
                                emit_z(g, kbs[i + 3], i + 3)
                        ob_ = g % 2
                        P.op("dve", lambda e, ob_=ob_: e.tensor_copy(osb[ob_][:], bank(OB)),
                             reads=[PS(OB)], writes=[("sos", ob_)])
                        P.dma("sp", attT[row:row + 128, g * 512:(g + 1) * 512], osb[ob_][:],
                              reads=[("sos", ob_)], writes=[("attT", 8 + 2 * hp, g), ("attT", 9 + 2 * hp, g)])

                for hp in range(NH // 2):
                    sb_pair(hp, hp % 2)
                P.barrier()

        if 3 in phases:
            phase_3()

        def phase_4():
            with contextlib.ExitStack() as ps4:
                wout = sb("wout", [128, 8, D], BF16, ps4)
                wrt = sb("wrt", [128, 8, 36], F32, ps4)
                at = [sb(f"at{i}", [128, 8, 512], F32, ps4) for i in range(2)]
                sq = [sb(f"sq{i}", [128, 512], F32, ps4) for i in range(2)]
                lnt = [sb(f"lnt{i}", [128, 512], F32, ps4) for i in range(2)]
                rsb = [sb(f"rsb{i}", [128, 512], F32, ps4) for i in range(2)]
                mixT = [sb(f"mixT{i}", [128, 8, 512], BF16, ps4) for i in range(2)]
                xb = [sb(f"exb{i}", [128, D], F32, ps4) for i in range(2)]
                x1b = [sb(f"ex1b{i}", [128, D], F32, ps4) for i in range(2)]
                yb = [sb(f"eyb{i}", [128, D], F32, ps4) for i in range(2)]
                junk = sb("junk4", [128, D], BF16, ps4)
                st = [[sb(f"est{n}{i}", [128, 1], F32, ps4) for i in range(2)] for n in ("ss", "ln", "rs")]
                h2b = [sb(f"h2b{i}", [128, 8, 512], BF16, ps4) for i in range(2)]
                h2f = [sb(f"h2f{i}", [128, 8, 128], F32, ps4) for i in range(2)]
                rt = {n: [sb(f"rt_{n}{i}", [128, w_], F32, ps4) for i in range(2)]
                      for n, w_ in (("lg", 36), ("gm", 1), ("gs", 4), ("ge", 4), ("gsum", 1), ("gg", 1), ("oh", 4),
                                    ("sel", 8), ("t8", 8), ("d21", 1), ("ed", 1), ("den", 1), ("w1", 1), ("w2", 1),
                                    ("m1", 8), ("m2", 8), ("wl", 8))}
                for k in range(8):
                    load_cast(wout[:, k, :], w_out[k * 128:(k + 1) * 128, :], 1024, ("pool", "dve")[k % 2], [("wout", k)])
                P.dma("sp", wrt[:], w_rt.rearrange("(k p) n -> p k n", p=128), writes=["wrt"])
                att_all = lambda g: [("attT", h, g) for h in range(16)]
                for g in range(NG):
                    gb_ = g % 2
                    P.dma("sp", at[gb_][:], attT[:, g * 512:(g + 1) * 512].rearrange("(c p) s -> p c s", p=128),
                          reads=att_all(g), writes=[("at", gb_)])
                    for grp in range(2):
                        pb = 6 + grp
                        for c4 in range(4):
                            c = grp * 4 + c4
                            si = c % 2
                            P.op("act", lambda e, c=c, si=si, gb_=gb_: e.activation(sq[si][:], at[gb_][:, c, :], AF.Square),
                                 reads=[("at", gb_)], writes=[("sq", si)])
                            P.op("pe", lambda e, si=si, pb=pb, c4=c4: e.matmul(
                                bank(pb), ones_f[:, :], sq[si][:], start=(c4 == 0), stop=(c4 == 3)),
                                reads=["ones_f", ("sq", si)], writes=[PS(pb)])
                        P.op("act", lambda e, pb=pb, grp=grp: e.activation(
                            lnt[grp][:], bank(pb), AF.Ln, bias=EPS, scale=1.0 / 512),
                            reads=[PS(pb)], writes=[("lnt", grp)])
                        P.op("act", lambda e, grp=grp: e.activation(rsb[grp][:], lnt[grp][:], AF.Exp, scale=-0.5),
                             reads=[("lnt", grp)], writes=[("rsb", grp)])
                        for c4 in range(4):
                            c = grp * 4 + c4
                            P.op("dve", lambda e, c=c, gb_=gb_, grp=grp: e.scalar_tensor_tensor(
                                mixT[gb_][:, c, :], at[gb_][:, c, :], vecT[:, 24 + c:25 + c], rsb[grp][:],
                                ALU.mult, ALU.mult),
                                reads=[("at", gb_), "vecT", ("rsb", grp)], writes=[("mixT", gb_, c)])
                    mix_all = [("mixT", gb_, c) for c in range(8)]

                    def s1(tt, g=g, gb_=gb_, mix_all=mix_all):
                        t = g * 4 + tt
                        b = t % 2
                        P.dma("sp", xb[b][:], x[t * 128:(t + 1) * 128, :], writes=[("exb", b)])
                        for half in range(2):
                            pb = 4 + half
                            for k in range(8):
                                P.op("pe", lambda e, k=k, half=half, pb=pb: e.matmul(
                                    bank(pb), mixT[gb_][:, k, tt * 128:(tt + 1) * 128],
                                    wout[:, k, half * 512:(half + 1) * 512], start=(k == 0), stop=(k == 7)),
                                    reads=mix_all + [("wout", k_) for k_ in range(8)], writes=[PS(pb)])
                            P.op("dve", lambda e, half=half, pb=pb: e.tensor_tensor(
                                x1b[b][:, half * 512:(half + 1) * 512], bank(pb),
                                gmix_bc[:, half * 512:(half + 1) * 512], ALU.mult),
                                reads=[PS(pb), ("gmix_bc", half)], writes=[("ex1b", b, half)])
                            P.op("pool", lambda e, half=half: e.tensor_tensor(
                                x1b[b][:, half * 512:(half + 1) * 512], x1b[b][:, half * 512:(half + 1) * 512],
                                xb[b][:, half * 512:(half + 1) * 512], ALU.add),
                                reads=[("ex1b", b, half), ("exb", b)], writes=[("ex1b", b, half)])
                        x1tok = [("ex1b", b, 0), ("ex1b", b, 1)]
                        P.dma("sp", x1[t * 128:(t + 1) * 128, :], x1b[b][:], reads=x1tok, writes=[("x1", t)])

                    def s2a(tt, g=g):
                        t = g * 4 + tt
                        b = t % 2
                        x1tok = [("ex1b", b, 0), ("ex1b", b, 1)]
                        norm_stats(x1b[b], x1tok, ("n4", b), st[0][b], st[1][b], st[2][b], junk, yb[b], ("eyb", b))

                    def s2b(tt, g=g, gb_=gb_):
                        t = g * 4 + tt
                        b = t % 2
                        norm_transp(16,
                                    [lambda j: h2b[gb_][:, j, tt * 128:(tt + 1) * 128], lambda j: h2f[b][:, j, :]],
                                    [("h2b", gb_, tt), ("h2f", b)], (2 * b, 2 * b + 1), yb[b], ("eyb", b), act_only=True)

                    def s3(tt, g=g):
                        t = g * 4 + tt
                        b = t % 2
                        R = {n: v_[b] for n, v_ in rt.items()}
                        pb = 6 + (t % 2)
                        for k in range(8):
                            P.op("pe", lambda e, k=k: e.matmul(
                                bank(pb, 0, 36), h2f[b][:, k, :], wrt[:, k, :], start=(k == 0), stop=(k == 7)),
                                reads=[("h2f", b), "wrt"], writes=[PS(pb)])

                        def V(n):
                            return ("rt", n, b)

                        def dve(fn, reads, writes):
                            P.op("dve", fn, reads=reads, writes=writes)

                        dve(lambda e: e.tensor_copy(R["lg"][:], bank(pb, 0, 36)), [PS(pb)], [V("lg")])
                        dve(lambda e: e.tensor_reduce(R["gm"][:], R["lg"][:, 0:4], AX.X, ALU.max), [V("lg")], [V("gm")])
                        dve(lambda e: e.tensor_scalar(R["gs"][:], R["lg"][:, 0:4], R["gm"][:, 0:1], None, ALU.subtract),
                            [V("lg"), V("gm")], [V("gs")])
                        P.op("act", lambda e: e.activation(R["ge"][:], R["gs"][:], AF.Exp, accum_out=R["gsum"][:]),
                             reads=[V("gs")], writes=[V("ge"), V("gsum")])
                        dve(lambda e: e.reciprocal(R["gg"][:], R["gsum"][:]), [V("gsum")], [V("gg")])
                        dve(lambda e: e.tensor_scalar(R["oh"][:], R["lg"][:, 0:4], R["gm"][:, 0:1], None, ALU.is_equal),
                            [V("lg"), V("gm")], [V("oh")])
                        dve(lambda e: e.tensor_scalar(R["sel"][:], R["lg"][:, 4:12], R["oh"][:, 0:1], None, ALU.mult),
                            [V("lg"), V("oh")], [V("sel")])
                        for gi in range(1, 4):
                            dve(lambda e, gi=gi: e.scalar_tensor_tensor(
                                R["sel"][:], R["lg"][:, 4 + 8 * gi:12 + 8 * gi], R["oh"][:, gi:gi + 1], R["sel"][:],
                                ALU.mult, ALU.add), [V("lg"), V("oh"), V("sel")], [V("sel")])
                        dve(lambda e: e.max(R["t8"][:], R["sel"][:]), [V("sel")], [V("t8")])
                        dve(lambda e: e.tensor_tensor(R["d21"][:], R["t8"][:, 1:2], R["t8"][:, 0:1], ALU.subtract),
                            [V("t8")], [V("d21")])
                        P.op("act", lambda e: e.activation(R["ed"][:], R["d21"][:], AF.Exp),
                             reads=[V("d21")], writes=[V("ed")])
                        dve(lambda e: e.tensor_scalar(R["den"][:], R["ed"][:], 1.0, None, ALU.add), [V("ed")], [V("den")])
                        dve(lambda e: e.reciprocal(R["den"][:], R["den"][:]), [V("den")], [V("den")])
                        dve(lambda e: e.tensor_tensor(R["w1"][:], R["den"][:], R["gg"][:], ALU.mult),
                            [V("den"), V("gg")], [V("w1")])
                        dve(lambda e: e.tensor_tensor(R["w2"][:], R["w1"][:], R["ed"][:], ALU.mult),
                            [V("w1"), V("ed")], [V("w2")])
                        dve(lambda e: e.tensor_scalar(R["m1"][:], R["sel"][:], R["t8"][:, 0:1], R["w1"][:, 0:1],
                                                      ALU.is_equal, ALU.mult),
                            [V("sel"), V("t8"), V("w1")], [V("m1")])
                        dve(lambda e: e.tensor_scalar(R["m2"][:], R["sel"][:], R["t8"][:, 1:2], R["w2"][:, 0:1],
                                                      ALU.is_equal, ALU.mult),
                            [V("sel"), V("t8"), V("w2")], [V("m2")])
                        dve(lambda e: e.tensor_tensor(R["wl"][:], R["m1"][:], R["m2"][:], ALU.add),
                            [V("m1"), V("m2")], [V("wl")])
                        for gi in range(4):
                            dve(lambda e, gi=gi: e.tensor_scalar(
                                Wr[:, t * 32 + gi * 8:t * 32 + gi * 8 + 8], R["wl"][:], R["oh"][:, gi:gi + 1], None,
                                ALU.mult), [V("wl"), V("oh")], [("Wr", t)])

                    for t0_ in (0, 2):
                        s1(t0_)
                        s1(t0_ + 1)
                        s2a(t0_)
                        s2a(t0_ + 1)
                        s2b(t0_)
                        s2b(t0_ + 1)
                        s3(t0_)
                        s3(t0_ + 1)
                    P.dma("sp", h2T[:, g * 512:(g + 1) * 512].rearrange("(c p) s -> p c s", p=128), h2b[gb_][:],
                          reads=[("h2b", gb_, tt) for tt in range(4)], writes=[("h2T", g)])
                P.barrier()

        if 4 in phases:
            phase_4()

        def phase_5():
            with contextlib.ExitStack() as ps5:
                GT = 1024
                NGT = S // GT
                hg = sb("hg", [128, 8, GT], BF16, ps5)
                acc = [sb(f"acc{i}", [128, D], F32, ps5) for i in range(GT // 128)]
                wg = [sb(f"wg{i}", [128, 8, DFF], BF16, ps5) for i in range(2)]
                wu = [sb(f"wu{i}", [128, 8, DFF], BF16, ps5) for i in range(2)]
                wd = [sb(f"wd{i}", [128, 4, D], BF16, ps5) for i in range(2)]
                sgt = [sb(f"sgt{i}", [128, 512], F32, ps5) for i in range(2)]
                hm = [sb(f"hm{i}", [128, 4, 512], BF16, ps5) for i in range(2)]
                x1b = [sb(f"fx1b{i}", [128, D], F32, ps5) for i in range(2)]
                ob = [sb(f"fob{i}", [128, D], F32, ps5) for i in range(2)]
                junk = sb("junk5", [128, D], BF16, ps5)
                st = [[sb(f"fst{n}{i}", [128, 1], F32, ps5) for i in range(2)] for n in ("ss", "ln", "rs")]
                nld = 0
                npd = [0]
                if True:
                    def loads(ex, wbuf):
                        v3 = ("p (k f) -> p k f", dict(f=512))
                        for kh_ in range(2):
                            load_cast(wg[wbuf][:, kh_ * 4:(kh_ + 1) * 4, :],
                                      w_gate[ex][kh_ * 512:(kh_ + 1) * 512, :].rearrange("(k p) f -> p k f", p=128),
                                      2048, "act", [("wg", wbuf, kh_)], view=v3)
                            load_cast(wu[wbuf][:, kh_ * 4:(kh_ + 1) * 4, :],
                                      w_up[ex][kh_ * 512:(kh_ + 1) * 512, :].rearrange("(k p) f -> p k f", p=128),
                                      2048, ("act", "dve")[kh_], [("wu", wbuf, kh_)], view=v3)
                        for kh_ in range(2):
                            load_cast(wd[wbuf][:, :, kh_ * 512:(kh_ + 1) * 512],
                                      w_down[ex][:, kh_ * 512:(kh_ + 1) * 512].rearrange("(k p) f -> p k f", p=128),
                                      2048, "pool", [("wd", wbuf, kh_)], view=v3)

                    def gate_up(ex, sg, wbuf, hb):
                        for fc in range(4):
                            for (wt_, wn, pb) in ((wg, "wg", 0 + 2 * (fc % 2)), (wu, "wu", 1 + 2 * (fc % 2))):
                                for k in range(8):
                                    P.op("pe", lambda e, wt_=wt_, k=k, fc=fc, pb=pb: e.matmul(
                                        bank(pb), wt_[wbuf][:, k, fc * 128:(fc + 1) * 128],
                                        hg[:, k, sg * 512:(sg + 1) * 512], start=(k == 0), stop=(k == 7)),
                                        reads=[(wn, wbuf, 0), (wn, wbuf, 1), "hg"], writes=[PS(pb)])
                            pg = 0 + 2 * (fc % 2)
                            pu = 1 + 2 * (fc % 2)
                            si = fc % 2
                            P.op("act", lambda e, si=si, pg=pg: e.activation(sgt[si][:], bank(pg), AF.Silu),
                                 reads=[PS(pg)], writes=[("sgt", si)])
                            P.op("dve", lambda e, fc=fc, si=si, pu=pu: e.tensor_tensor(
                                hm[hb][:, fc, :], sgt[si][:], bank(pu), ALU.mult),
                                reads=[("sgt", si), PS(pu)], writes=[("hm", hb, fc)])

                    def down(ex, sg, wbuf, hb, G):
                        hm_all = [("hm", hb, fc) for fc in range(4)]
                        for tt in range(4):
                            tl = sg * 4 + tt
                            tg = G * (GT // 128) + tl
                            for half in range(2):
                                pb = 4 + npd[0] % 4
                                npd[0] += 1
                                for fc in range(4):
                                    P.op("pe", lambda e, fc=fc, tt=tt, half=half, pb=pb: e.matmul(
                                        bank(pb), hm[hb][:, fc, tt * 128:(tt + 1) * 128],
                                        wd[wbuf][:, fc, half * 512:(half + 1) * 512], start=(fc == 0), stop=(fc == 3)),
                                        reads=hm_all + [("wd", wbuf, half)], writes=[PS(pb)])
                                wcol = Wr[:, tg * 32 + ex:tg * 32 + ex + 1]
                                dst = acc[tl][:, half * 512:(half + 1) * 512]
                                if ex == 0:
                                    P.op("dve", lambda e, dst=dst, pb=pb, wcol=wcol: e.tensor_scalar(
                                        dst, bank(pb), wcol, None, ALU.mult),
                                        reads=[PS(pb), ("Wr", tg)], writes=[("acc", tl, half)])
                                else:
                                    P.op("dve", lambda e, dst=dst, pb=pb, wcol=wcol: e.scalar_tensor_tensor(
                                        dst, bank(pb), wcol, dst, ALU.mult, ALU.add),
                                        reads=[PS(pb), ("Wr", tg), ("acc", tl, half)], writes=[("acc", tl, half)])

                    def final_norm(G):
                        for tl in range(GT // 128):
                            tg = G * (GT // 128) + tl
                            b = tg % 2
                            P.dma("sp", x1b[b][:], x1[tg * 128:(tg + 1) * 128, :], reads=[("x1", tg)], writes=[("fx1b", b)])
                            for half in range(2):
                                hs_ = slice(half * 512, (half + 1) * 512)
                                fe = ("pool", "dve")[half]
                                P.op(fe, lambda e, tl=tl, hs_=hs_: e.tensor_tensor(
                                    acc[tl][:, hs_], acc[tl][:, hs_], gffn_bc[:, hs_], ALU.mult),
                                    reads=[("acc", tl, half), ("gffn_bc", half)], writes=[("acc", tl, half)])
                                P.op(fe, lambda e, tl=tl, hs_=hs_, b=b: e.tensor_tensor(
                                    acc[tl][:, hs_], acc[tl][:, hs_], x1b[b][:, hs_], ALU.add),
                                    reads=[("acc", tl, half), ("fx1b", b)], writes=[("acc", tl, half)])
                            a_all = [("acc", tl, 0), ("acc", tl, 1)]
                            P.op("act", lambda e, tl=tl, b=b: e.activation(junk[:], acc[tl][:], AF.Square, accum_out=st[0][b][:]),
                                 reads=a_all, writes=[("fjunk",), ("fss", b)])
                            P.op("act", lambda e, b=b: e.activation(st[1][b][:], st[0][b][:], AF.Ln, bias=EPS, scale=1.0 / D),
                                 reads=[("fss", b)], writes=[("fln", b)])
                            P.op("act", lambda e, b=b: e.activation(st[2][b][:], st[1][b][:], AF.Exp, scale=-0.5),
                                 reads=[("fln", b)], writes=[("frs", b)])
                            P.op("dve", lambda e, tl=tl, b=b: e.scalar_tensor_tensor(
                                ob[b][:], acc[tl][:], st[2][b][:, 0:1], gfin_bc[:], ALU.mult, ALU.mult),
                                reads=a_all + [("frs", b), ("gfin_bc", 0), ("gfin_bc", 1)], writes=[("fob", b)])
                            P.dma("sp", out[tg * 128:(tg + 1) * 128, :], ob[b][:], reads=[("fob", b)], writes=[("out", tg)])

                    pending = None
                    nu = 0
                    for G in range(NGT):
                        for ex in range(NEXP):
                            wbuf = nld % 2
                            nld += 1
                            if ex == 0:
                                P.dma("sp", hg[:], h2T[:, G * GT:(G + 1) * GT].rearrange("(c p) s -> p c s", p=128),
                                      reads=[("h2T", G * 4 + i) for i in range(4)], writes=["hg"])
                            loads(ex, wbuf)
                            for sg in range(GT // 512):
                                gate_up(ex, sg, wbuf, nu % 2)
                                if pending is not None:
                                    down(*pending)
                                    if pending[4] != G:
                                        final_norm(pending[4])
                                pending = (ex, sg, wbuf, nu % 2, G)
                                nu += 1
                    down(*pending)
                    final_norm(pending[4])
                P.barrier()

        if 5 in phases:
            phase_5()

        P.finish()
    return nc


_CACHE = {}


def _get_nc(S, phases=(0, 1, 2, 3, 4, 5), dbg=False):
    key = (S, tuple(phases), dbg)
    if key not in _CACHE:
        _CACHE[key] = build_nc(S, phases, dbg)
    return _CACHE[key]


def make_in_maps(inputs, S, n):
    f = np.float32
    consts = _host_consts()
    shared = {
        "w_ada": np.ascontiguousarray(inputs["w_ada"][0], f),
        "b_ada": np.ascontiguousarray(inputs["b_ada"][0:1], f),
        "g_mix": np.ascontiguousarray(inputs["g_mix"][0:1], f),
        "w_in": np.ascontiguousarray(inputs["w_in"][0], f),
        "g_dil_out": np.ascontiguousarray(inputs["g_dil_out"][0:1], f),
        "g_sb_out": np.ascontiguousarray(inputs["g_sb_out"][0:1], f),
        "w_out": np.ascontiguousarray(inputs["w_out"][0], f),
        "g_ffn": np.ascontiguousarray(inputs["g_ffn"][0:1], f),
        "w_rt": np.ascontiguousarray(np.concatenate([inputs["w_group"][0], inputs["w_expert"][0]], axis=1), f),
        "w_gate": np.ascontiguousarray(inputs["w_gate"][0], f),
        "w_up": np.ascontiguousarray(inputs["w_up"][0], f),
        "w_down": np.ascontiguousarray(inputs["w_down"][0], f),
        "g_final": np.ascontiguousarray(np.asarray(inputs["g_final"]).reshape(1, D), f),
    }
    for k, v in consts.items():
        shared["k_" + k] = v
    maps = []
    for b in range(n):
        m = dict(shared)
        m["x"] = np.ascontiguousarray(inputs["x"][b, :S], f)
        m["c"] = np.ascontiguousarray(inputs["c"][b:b + 1], f)
        maps.append(m)
    return maps


def kernel(**inputs):
    inputs = {k: np.asarray(v) for k, v in inputs.items()}
    B, S, _ = inputs["x"].shape
    nc = _get_nc(S)
    maps = make_in_maps(inputs, S, B)
    res = run_bass_kernel_spmd(nc, maps, core_ids=list(range(B)))
    return np.stack([np.asarray(r["out"], dtype=np.float32) for r in res.results], axis=0)
```

```python
import contextlib
import numpy as np
import ml_dtypes
import concourse.bass as bass
import concourse.mybir as mybir
from concourse.bass_utils import run_bass_kernel_spmd

F32 = mybir.dt.float32
BF16 = mybir.dt.bfloat16
I32 = mybir.dt.int32
AF = mybir.ActivationFunctionType
ALU = mybir.AluOpType
AX = mybir.AxisListType


class _Op:
    __slots__ = ("eng", "fn", "deps", "needed", "is_dma", "sem", "val", "pos")

    def __init__(self, eng, fn, is_dma):
        self.eng = eng
        self.fn = fn
        self.deps = set()
        self.needed = False
        self.is_dma = is_dma
        self.sem = None
        self.val = 0
        self.pos = 0


class Prog:
    ENGS = ("pe", "act", "dve", "pool", "sp")
    NSLOT = 16

    def __init__(self, nc, es):
        self.nc = nc
        self.streams = {e: [] for e in self.ENGS}
        self.last_writer = {}
        self.readers = {}
        self.esem = {e: es.enter_context(nc.semaphore("sem_" + e)) for e in self.ENGS}
        self.dsem = {
            q: [es.enter_context(nc.semaphore(f"dsem_{q}{i}")) for i in range(self.NSLOT)]
            for q in ("sp", "pool", "act")
        }
        self.ndma = {q: 0 for q in self.dsem}
        self.dma_last = {q: [None] * self.NSLOT for q in self.dsem}

    def _add(self, op, reads, writes):
        eng = op.eng
        for r in reads:
            w = self.last_writer.get(r)
            if w is not None:
                if w.is_dma or w.eng != eng or eng != "pe":
                    op.deps.add(w)
            if isinstance(r, tuple) and r and r[0] == "ps":
                rd = self.readers.get(r)
                if rd is not None:
                    for e2, o2 in rd[0].items():
                        if e2 != eng:
                            op.deps.add(o2)
        for r in writes:
            w = self.last_writer.get(r)
            if w is not None and (w.is_dma or w.eng != eng):
                op.deps.add(w)
            rd = self.readers.get(r)
            if rd is not None:
                for e2, o2 in rd[0].items():
                    if e2 != eng and o2 is not op:
                        op.deps.add(o2)
                for o2 in rd[1]:
                    if o2 is not op:
                        op.deps.add(o2)
        for r in reads:
            rd = self.readers.setdefault(r, ({}, []))
            if op.is_dma:
                rd[1].append(op)
            else:
                rd[0][eng] = op
        for r in writes:
            self.last_writer[r] = op
            self.readers[r] = ({}, [])
        op.deps.discard(op)
        self.streams[eng].append(op)
        return op

    def op(self, eng, fn, reads=(), writes=()):
        return self._add(_Op(eng, fn, False), reads, writes)

    def dma(self, q, out, in_, reads=(), writes=(), **kw):
        o = _Op(q, lambda e: e.dma_start(out=out, in_=in_, **kw), True)
        i = self.ndma[q]
        self.ndma[q] = i + 1
        o.sem = self.dsem[q][i % self.NSLOT]
        o.val = 16 * (i // self.NSLOT + 1)
        prev = self.dma_last[q][i % self.NSLOT]
        if prev is not None:
            o.deps.add(prev)
        self.dma_last[q][i % self.NSLOT] = o
        return self._add(o, reads, writes)

    def barrier(self):
        lasts = []
        for e in self.ENGS:
            for o in reversed(self.streams[e]):
                if not o.is_dma and o.fn is not None:
                    lasts.append(o)
                    break
        for q in self.dsem:
            for o in self.dma_last[q]:
                if o is not None:
                    lasts.append(o)
        for e in self.ENGS:
            o = _Op(e, None, False)
            o.deps = set(x for x in lasts if x.is_dma or x.eng != e)
            self.streams[e].append(o)

    def finish(self):
        self.barrier()
        for e in self.ENGS:
            for o in self.streams[e]:
                for d in o.deps:
                    d.needed = True
        for e in self.ENGS:
            c = 0
            for o in self.streams[e]:
                if o.is_dma or o.fn is None:
                    continue
                if o.needed:
                    c += 1
                    o.sem = self.esem[e]
                    o.val = c
        nc = self.nc
        with nc.Block() as block:
            for e, deco in (("pe", block.tensor), ("act", block.scalar), ("dve", block.vector),
                            ("pool", block.gpsimd), ("sp", block.sync)):
                stream = self.streams[e]

                def body(eng, stream=stream):
                    waited = {}
                    for o in stream:
                        need = {}
                        for d in o.deps:
                            k = id(d.sem)
                            if waited.get(k, 0) >= d.val:
                                continue
                            if k not in need or need[k][1] < d.val:
                                need[k] = (d.sem, d.val)
                        for k, (s, v) in need.items():
                            eng.wait_ge(s, v)
                            waited[k] = v
                        if o.fn is None:
                            continue
                        ins = o.fn(eng)
                        if o.is_dma:
                            ins.then_inc(o.sem, 16)
                        elif o.needed:
                            ins.then_inc(o.sem, 1)

                deco(body)


D = 1024
HD = 64
NH = 8
NEXP = 32
DFF = 512
EPS = 1e-6
PATTERNS = ((128, 1), (512, 4), (2048, 16))
MASKV = -240.0
NEG = -1.0e30


def _host_consts():
    f = np.float32
    c = {}
    c["ident"] = np.eye(128, dtype=f)
    j = np.arange(128)[:, None]
    s = np.arange(128)[None, :]
    c["negM"] = np.where(j >= s, -1.0, 0.0).astype(f)
    c["negN"] = np.where(j < s, -1.0, 0.0).astype(f)
    c["negI"] = (-np.eye(128)).astype(f)
    mb = np.zeros((128, 4, 512), f)
    for v in range(4):
        for cq in range(4):
            blk = np.zeros((128, 128), f)
            if cq < v:
                blk[:] = MASKV
            elif cq == v:
                blk = np.where(j < s, 0.0, MASKV).astype(f)
            mb[:, v, cq * 128:(cq + 1) * 128] = blk
    c["mb"] = mb.reshape(128, 2048)
    slopes = np.array([2.0 ** (-8.0 * (i + 1) / NH) for i in range(NH)], dtype=f)
    db = np.zeros((128, NH, 3, 256), f)
    for h in range(NH):
        for p, (w, d) in enumerate(PATTERNS):
            i = np.arange(128)[None, :]
            jj = np.arange(128)[:, None]
            cur = np.where(jj <= i, -slopes[h] * ((i - jj) * d).astype(f), NEG)
            prev = np.where(jj >= i, -slopes[h] * ((i + 128 - jj) * d).astype(f), NEG)
            db[:, h, p, 0:128] = cur
            db[:, h, p, 128:256] = prev
    c["dbias"] = db.reshape(128, NH * 3 * 256)
    sh = np.zeros((128, 64), f)
    sh[64 + np.arange(64), np.arange(64)] = 1.0
    c["shiftm"] = sh
    return c


def build_nc(S, phases=(0, 1, 2, 3, 4, 5), dbg=False):
    NT = S // 128
    NG = S // 512
    nc = bass.Bass("TRN2", target_bir_lowering=False)

    def din(name, shape, dt=F32):
        return nc.dram_tensor(name, list(shape), dt, kind="ExternalInput").ap()

    def dscr(name, shape, dt):
        return nc.dram_tensor(name, list(shape), dt, kind=("ExternalOutput" if dbg else "Internal")).ap()

    x = din("x", [S, D])
    cvec = din("c", [1, D])
    w_ada = din("w_ada", [D, 6 * D])
    b_ada = din("b_ada", [1, 6 * D])
    g_mix = din("g_mix", [1, D])
    w_in = din("w_in", [D, 3 * D])
    g_dil = din("g_dil_out", [1, 512])
    g_sb = din("g_sb_out", [1, 512])
    w_out = din("w_out", [D, D])
    g_ffn = din("g_ffn", [1, D])
    w_rt = din("w_rt", [D, 36])
    w_gate = din("w_gate", [NEXP, D, DFF])
    w_up = din("w_up", [NEXP, D, DFF])
    w_down = din("w_down", [NEXP, DFF, D])
    g_fin = din("g_final", [1, D])
    k_ident = din("k_ident", [128, 128])
    k_negM = din("k_negM", [128, 128])
    k_negN = din("k_negN", [128, 128])
    k_negI = din("k_negI", [128, 128])
    k_mb = din("k_mb", [128, 2048])
    k_dbias = din("k_dbias", [128, NH * 3 * 256])
    k_shiftm = din("k_shiftm", [128, 64])
    out = nc.dram_tensor("out", [S, D], F32, kind="ExternalOutput").ap()

    qT = dscr("qT", [D, S], BF16)
    kT = dscr("kT", [D, S], BF16)
    vv = dscr("vv", [S, D], BF16)
    attT = dscr("attT", [D, S], F32)
    x1 = dscr("x1", [S, D], F32)
    h2T = dscr("h2T", [D, S], BF16)

    with contextlib.ExitStack() as es:
        def sb(name, shape, dt, stack=es):
            return stack.enter_context(nc.sbuf_tensor(name, list(shape), dt))

        P = Prog(nc, es)
        psum = es.enter_context(nc.psum_tensor("psum", [128, 4096], F32))

        def bank(b, lo=0, hi=512, p0=0, p1=128):
            return psum[p0:p1, b * 512 + lo:b * 512 + hi]

        def PS(b):
            return ("ps", b)

        ident = sb("ident", [128, 128], F32)
        identb = sb("identb", [128, 128], BF16)
        negIb = sb("negIb", [128, 128], BF16)
        negMb = sb("negMb", [128, 128], BF16)
        negNb = sb("negNb", [128, 128], BF16)
        mbb = sb("mbb", [128, 2048], BF16)
        shiftm = sb("shiftm", [128, 64], F32)
        ones_f = sb("ones_f", [128, 128], F32)
        ones_b = sb("ones_b", [128, 64], BF16)
        vecT = sb("vecT", [128, 32], F32)
        AB = sb("AB", [128, 32], F32)
        gmix_bc = sb("gmix_bc", [128, D], F32)
        gffn_bc = sb("gffn_bc", [128, D], F32)
        gfin_bc = sb("gfin_bc", [128, D], F32)
        Wr = sb("Wr", [128, NT * 32], F32)

        stg = [sb(f"stg{i}", [128, 2048], F32) for i in range(4)]
        nstg = {"act": 0}

        def load_cast(dst, src, n, eng, dtoks, view=None, rd=()):
            if eng == "act":
                i = nstg["act"] % 2
                nstg["act"] += 1
            else:
                i = 2 if eng == "pool" else 3
            sv = stg[i][:, 0:n]
            if view is not None:
                sv = sv.rearrange(view[0], **view[1])
            P.dma("sp", sv, src, reads=list(rd), writes=[("stg", i)])
            if eng == "act":
                P.op("act", lambda e: e.activation(dst, sv, AF.Copy), reads=[("stg", i)], writes=list(dtoks))
            else:
                P.op(eng, lambda e: e.tensor_copy(dst, sv), reads=[("stg", i)], writes=list(dtoks))

        P.dma("sp", ident[:], k_ident, writes=["ident"])
        P.dma("sp", shiftm[:], k_shiftm, writes=["shiftm"])
        load_cast(identb[:], k_ident, 128, "dve", ["identb"])
        load_cast(negIb[:], k_negI, 128, "dve", ["negIb"])
        load_cast(negMb[:], k_negM, 128, "dve", ["negMb"])
        load_cast(negNb[:], k_negN, 128, "dve", ["negNb"])
        load_cast(mbb[:], k_mb, 2048, "dve", ["mbb"])
        P.op("dve", lambda e: e.memset(ones_f[:], 1.0), writes=["ones_f"])
        P.op("dve", lambda e: e.memset(ones_b[:], 1.0), writes=["ones_b"])

        def phase_0():
            with contextlib.ExitStack() as ps0:
                vecs = sb("vecs", [32, 128], F32, ps0)
                sc = sb("sc", [128, 8], F32, ps0)
                wa = [sb(f"wa{i}", [128, 8, 512], F32, ps0) for i in range(2)]
                brow = sb("brow", [1, 6 * D], F32, ps0)
                mrow = sb("mrow", [1, 6 * D], F32, ps0)
                gfrow = sb("gfrow", [1, D], F32, ps0)
                for r0, src, n in ((0, cvec, 8), (8, g_mix, 8), (16, g_ffn, 8), (24, g_dil, 4), (28, g_sb, 4)):
                    P.dma("sp", vecs[r0:r0 + n, :], src.rearrange("o (k p) -> (o k) p", p=128),
                          writes=[("vecs", r0)])
                P.dma("sp", brow[:], b_ada, writes=["brow"])
                P.dma("sp", gfrow[:], g_fin, writes=["gfrow"])
                P.op("pe", lambda e: e.transpose(bank(0, 0, 32), vecs[:, :], ident[0:32, 0:32]),
                     reads=[("vecs", r) for r in (0, 8, 16, 24, 28)] + ["ident"], writes=[PS(0)])
                P.op("dve", lambda e: e.tensor_copy(vecT[:], bank(0, 0, 32)), reads=[PS(0)], writes=["vecT"])
                P.op("act", lambda e: e.activation(sc[:], vecT[:, 0:8], AF.Silu), reads=["vecT"], writes=["sc"])
                for blk in range(12):
                    b = blk % 2
                    P.dma("sp", wa[b][:], w_ada[:, blk * 512:(blk + 1) * 512].rearrange("(k p) n -> p k n", p=128),
                          writes=[("wa", b)])
                    pb = 1 + b
                    for k in range(8):
                        P.op("pe", lambda e, k=k, b=b, pb=pb: e.matmul(
                            bank(pb, 0, 512, 0, 1), sc[:, k:k + 1], wa[b][:, k, :], start=(k == 0), stop=(k == 7)),
                            reads=["sc", ("wa", b)], writes=[PS(pb)])
                    P.op("dve", lambda e, blk=blk, pb=pb: e.tensor_tensor(
                        mrow[0:1, blk * 512:(blk + 1) * 512], bank(pb, 0, 512, 0, 1),
                        brow[0:1, blk * 512:(blk + 1) * 512], ALU.add),
                        reads=[PS(pb), "brow"], writes=[("mrow", blk)])
                mrow_all = [("mrow", i) for i in range(12)]
                for si, seg in enumerate((0, 1, 3, 4)):
                    for k in range(8):
                        col = si * 8 + k
                        P.op("pe", lambda e, seg=seg, k=k, col=col: e.matmul(
                            bank(3, col, col + 1), mrow[0:1, seg * D + k * 128: seg * D + (k + 1) * 128],
                            ones_f[0:1, 0:1], start=True, stop=True),
                            reads=mrow_all + ["ones_f"], writes=[PS(3)])
                P.op("dve", lambda e: e.scalar_tensor_tensor(AB[:, 0:8], bank(3, 8, 16), 1.0, vecT[:, 8:16],
                                                             ALU.add, ALU.mult),
                     reads=[PS(3), "vecT"], writes=[("AB", 0)])
                P.op("dve", lambda e: e.tensor_copy(AB[:, 8:16], bank(3, 0, 8)), reads=[PS(3)], writes=[("AB", 1)])
                P.op("dve", lambda e: e.scalar_tensor_tensor(AB[:, 16:24], bank(3, 24, 32), 1.0, vecT[:, 16:24],
                                                             ALU.add, ALU.mult),
                     reads=[PS(3), "vecT"], writes=[("AB", 2)])
                P.op("dve", lambda e: e.tensor_copy(AB[:, 24:32], bank(3, 16, 24)), reads=[PS(3)], writes=[("AB", 3)])
                for (dst, nm, row, off) in ((gmix_bc, "gmix_bc", mrow, 2 * D), (gffn_bc, "gffn_bc", mrow, 5 * D),
                                            (gfin_bc, "gfin_bc", gfrow, 0)):
                    for half in range(2):
                        pb = 4 + half
                        P.op("pe", lambda e, row=row, off=off, half=half, pb=pb: e.matmul(
                            bank(pb), ones_f[0:1, 0:128], row[0:1, off + half * 512: off + (half + 1) * 512],
                            start=True, stop=True),
                            reads=mrow_all + ["gfrow", "ones_f"], writes=[PS(pb)])
                        P.op("act", lambda e, dst=dst, half=half, pb=pb: e.activation(
                            dst[:, half * 512:(half + 1) * 512], bank(pb), AF.Copy),
                            reads=[PS(pb)], writes=[(nm, half)])
                P.barrier()

        if 0 in phases:
            phase_0()

        AB_all = [("AB", i) for i in range(4)]

        def norm_stats(xt, xt_toks, tag, ss, lnv, rstd, junk, y, y_tok):
            P.op("act", lambda e: e.activation(junk[:], xt[:], AF.Square, accum_out=ss[:]),
                 reads=list(xt_toks), writes=[(tag, "junk"), (tag, "ss")])
            P.op("act", lambda e: e.activation(lnv[:], ss[:], AF.Ln, bias=EPS, scale=1.0 / D),
                 reads=[(tag, "ss")], writes=[(tag, "lnv")])
            P.op("act", lambda e: e.activation(rstd[:], lnv[:], AF.Exp, scale=-0.5),
                 reads=[(tag, "lnv")], writes=[(tag, "rstd")])
            P.op("dve", lambda e: e.tensor_scalar(y[:], xt[:], rstd[:, 0:1], None, ALU.mult),
                 reads=list(xt_toks) + [(tag, "rstd")], writes=[y_tok])

        def norm_transp(Acol, dsts, dst_toks, pbanks, y, y_tok, act_only=False):
            for j in range(8):
                pb = pbanks[j // 4]
                P.op("pe", lambda e, j=j, pb=pb: e.transpose(
                    bank(pb, (j % 4) * 128, (j % 4 + 1) * 128), y[:, j * 128:(j + 1) * 128], ident[:]),
                    reads=[y_tok, "ident"], writes=[PS(pb)])
            for j in range(8):
                pb = pbanks[j // 4]
                src = bank(pb, (j % 4) * 128, (j % 4 + 1) * 128)
                for di, (dst_fn, dtok) in enumerate(zip(dsts, dst_toks)):
                    use_act = act_only or ((j // 4 + di) % 2 == 0)
                    if use_act:
                        P.op("act", lambda e, j=j, src=src, dst_fn=dst_fn: e.activation(
                            dst_fn(j), src, AF.Identity, bias=AB[:, Acol + 8 + j:Acol + 9 + j],
                            scale=AB[:, Acol + j:Acol + j + 1]),
                            reads=[PS(pb)] + AB_all, writes=[dtok])
                    else:
                        P.op("dve", lambda e, j=j, src=src, dst_fn=dst_fn: e.tensor_scalar(
                            dst_fn(j), src, AB[:, Acol + j:Acol + j + 1], AB[:, Acol + 8 + j:Acol + 9 + j],
                            ALU.mult, ALU.add),
                            reads=[PS(pb)] + AB_all, writes=[dtok])

        def phase_1():
            with contextlib.ExitStack() as ps1:
                win = sb("win", [128, 8, 3 * D], BF16, ps1)
                xb = [sb(f"xb{i}", [128, D], F32, ps1) for i in range(2)]
                yb = [sb(f"yb{i}", [128, D], F32, ps1) for i in range(2)]
                junk = sb("junk1", [128, D], BF16, ps1)
                st = [[sb(f"st{n}{i}", [128, 1], F32, ps1) for i in range(2)] for n in ("ss", "ln", "rs")]
                hT = [sb(f"hT{i}", [128, 8, 512], BF16, ps1) for i in range(2)]
                qst = [sb(f"qst{i}", [128, 512], BF16, ps1) for i in range(4)]
                vst = [sb(f"vst{i}", [128, D], BF16, ps1) for i in range(2)]
                for k in range(8):
                    for half in range(2):
                        load_cast(win[:, k, half * 1536:(half + 1) * 1536],
                                  w_in[k * 128:(k + 1) * 128, half * 1536:(half + 1) * 1536], 1536,
                                  ("pool", "dve")[half], [("win", k, half)])
                win_all = [("win", k, half) for k in range(8) for half in range(2)]
                qk = []
                for i in range(4):
                    qk.append((0 + 128 * i, qT, 128 * i, 0.125))
                    qk.append((512 + 128 * i, kT, 128 * i, 1.0))
                    qk.append((1536 + 128 * i, qT, 512 + 128 * i, 0.125))
                    qk.append((2048 + 128 * i, kT, 512 + 128 * i, 1.0))
                nev = 0
                for g in range(NG):
                    hb = g % 2
                    for t0_ in (0, 2):
                        for tt in (t0_, t0_ + 1):
                            t = g * 4 + tt
                            b = t % 2
                            P.dma("sp", xb[b][:], x[t * 128:(t + 1) * 128, :], writes=[("xb", b)])
                            norm_stats(xb[b], [("xb", b)], ("n1", b), st[0][b], st[1][b], st[2][b], junk, yb[b], ("yb", b))
                        for tt in (t0_, t0_ + 1):
                            t = g * 4 + tt
                            b = t % 2
                            norm_transp(0, [lambda j, hb=hb, tt=tt: hT[hb][:, j, tt * 128:(tt + 1) * 128]],
                                        [("hT", hb, tt)], (2 * b, 2 * b + 1), yb[b], ("yb", b))
                    hT_all = [("hT", hb, tt) for tt in range(4)]
                    for ci, (col, dst, row, scl) in enumerate(qk):
                        pb = 4 + (nev % 4)
                        for k in range(8):
                            P.op("pe", lambda e, k=k, col=col, hb=hb, pb=pb: e.matmul(
                                bank(pb), win[:, k, col:col + 128], hT[hb][:, k, :], start=(k == 0), stop=(k == 7)),
                                reads=win_all + hT_all, writes=[PS(pb)])
                        sbi = nev % 4
                        if nev % 2 == 0:
                            P.op("act", lambda e, sbi=sbi, pb=pb, scl=scl: e.activation(
                                qst[sbi][:], bank(pb), AF.Copy, scale=scl), reads=[PS(pb)], writes=[("qst", sbi)])
                        else:
                            P.op("dve", lambda e, sbi=sbi, pb=pb, scl=scl: e.tensor_scalar(
                                qst[sbi][:], bank(pb), scl, None, ALU.mult), reads=[PS(pb)], writes=[("qst", sbi)])
                        P.dma("sp", dst[row:row + 128, g * 512:(g + 1) * 512], qst[sbi][:],
                              reads=[("qst", sbi)], writes=[("qk", id(dst), row, g)])
                        nev += 1
                    for tt in range(4):
                        t = g * 4 + tt
                        vb = t % 2
                        for half, col in enumerate((1024, 2560)):
                            pb = 4 + (nev % 4)
                            for k in range(8):
                                P.op("pe", lambda e, k=k, col=col, hb=hb, tt=tt, pb=pb: e.matmul(
                                    bank(pb), hT[hb][:, k, tt * 128:(tt + 1) * 128], win[:, k, col:col + 512],
                                    start=(k == 0), stop=(k == 7)),
                                    reads=win_all + hT_all, writes=[PS(pb)])
                            if nev % 2 == 0:
                                P.op("act", lambda e, vb=vb, half=half, pb=pb: e.activation(
                                    vst[vb][:, half * 512:(half + 1) * 512], bank(pb), AF.Copy),
                                    reads=[PS(pb)], writes=[("vst", vb, half)])
                            else:
                                P.op("dve", lambda e, vb=vb, half=half, pb=pb: e.tensor_copy(
                                    vst[vb][:, half * 512:(half + 1) * 512], bank(pb)),
                                    reads=[PS(pb)], writes=[("vst", vb, half)])
                            nev += 1
                        P.dma("sp", vv[t * 128:(t + 1) * 128, :], vst[vb][:],
                              reads=[("vst", vb, 0), ("vst", vb, 1)], writes=[("vv", t)])
                P.barrier()

        if 1 in phases:
            phase_1()

        def phase_2():
            with contextlib.ExitStack() as ps2:
                qh = [sb(f"dqh{i}", [64, S], BF16, ps2) for i in range(2)]
                kh = [sb(f"dkh{i}", [64, S], BF16, ps2) for i in range(2)]
                vp1 = [sb(f"dvp_{p}", [128, NT, 128], BF16, ps2) for p in range(3)]
                vp = [vp1, vp1]
                for p_ in range(3):
                    P.op("dve", lambda e, p_=p_: e.memset(vp1[p_][:, :, 64:128], 1.0), writes=[("dvp1", p_)])
                dbs = [sb(f"dbs{i}", [128, 3 * 256], F32, ps2) for i in range(2)]
                ssb = [sb(f"dss{i}", [128, 256], F32, ps2) for i in range(4)]
                wsb = [sb(f"dws{i}", [128, 256], BF16, ps2) for i in range(4)]
                accs = sb("daccs", [128, 2048], F32, ps2)
                lnd = [sb(f"dlnd{i}", [64, 512], F32, ps2) for i in range(2)]
                rdn = [sb(f"drdn{i}", [64, 512], F32, ps2) for i in range(2)]
                ost = [sb(f"dost{i}", [64, 512], F32, ps2) for i in range(2)]
                nsc = 0
                for h in range(NH):
                    hb = h % 2
                    P.dma("sp", qh[hb][:], qT[h * 64:(h + 1) * 64, :],
                          reads=[("qk", id(qT), (h // 2) * 128, g) for g in range(NG)], writes=[("dqh", hb)])
                    P.dma("sp", kh[hb][:], kT[h * 64:(h + 1) * 64, :],
                          reads=[("qk", id(kT), (h // 2) * 128, g) for g in range(NG)], writes=[("dkh", hb)])
                    P.dma("sp", dbs[hb][:], k_dbias[:, h * 768:(h + 1) * 768], writes=[("dbs", hb)])
                    for p, (w_, d) in enumerate(PATTERNS):
                        nb = S // (128 * d)
                        if d == 1:
                            P.dma("sp", vp[hb][p][:, :, 0:64], vv[:, h * 64:(h + 1) * 64].rearrange("(t j) e -> j t e", j=128),
                                  reads=[("vv", t) for t in range(NT)], writes=[("dvp", 0, p, 0)])
                        else:
                            for b_ in range(nb):
                                P.dma("sp", vp[hb][p][:, b_ * d:(b_ + 1) * d, 0:64],
                                      vv[b_ * 128 * d:(b_ + 1) * 128 * d, h * 64:(h + 1) * 64].rearrange(
                                          "(j r) e -> j r e", r=d),
                                      reads=[("vv", t) for t in range(NT)], writes=[("dvp", 0, p, b_)])
                    for sbk in range(S // 2048):
                        first_in_bank = [True] * 4
                        combos = []
                        for p, (w_, d) in enumerate(PATTERNS):
                            for cb in range(16):
                                if d == 1:
                                    blk, r = sbk * 16 + cb, 0
                                elif d == 4:
                                    blk, r = sbk * 4 + cb // 4, cb % 4
                                else:
                                    blk, r = sbk, cb
                                combos.append((p, d, cb, blk, r))

                        def stage_a(p, d, cb, blk, r, si, pb, hb=hb):
                            has_prev = blk > 0
                            wdt = 256 if has_prev else 128
                            c0 = blk * 128 * d + r
                            cur = slice(c0, c0 + 127 * d + 1, d)
                            prv = slice(c0 - 128 * d, c0 - d + 1, d)
                            rd = [("dqh", hb), ("dkh", hb)]
                            P.op("pe", lambda e: e.matmul(
                                bank(pb, 0, 128), kh[hb][:, cur], qh[hb][:, cur], start=True, stop=True),
                                reads=rd, writes=[PS(pb)])
                            if has_prev:
                                P.op("pe", lambda e: e.matmul(
                                    bank(pb, 128, 256), kh[hb][:, prv], qh[hb][:, cur], start=True, stop=True),
                                    reads=rd, writes=[PS(pb)])
                            P.op("dve", lambda e: e.tensor_tensor(
                                ssb[si][:, 0:wdt], bank(pb, 0, wdt), dbs[hb][:, p * 256:p * 256 + wdt], ALU.add),
                                reads=[PS(pb), ("dbs", hb)], writes=[("dss", si)])
                            P.op("act", lambda e: e.activation(wsb[si][:, 0:wdt], ssb[si][:, 0:wdt], AF.Exp),
                                 reads=[("dss", si)], writes=[("dws", si)])

                        def stage_b(p, d, cb, blk, r, si, pb, hb=hb, first_in_bank=first_in_bank):
                            has_prev = blk > 0
                            combo = blk * d + r
                            pcombo = (blk - 1) * d + r
                            for half in range(2 if has_prev else 1):
                                vc = combo if half == 0 else pcombo
                                npc = 4 if d == 16 else 1
                                for pc in range(npc):
                                    if d == 16:
                                        bk = pc
                                        qs = slice(half * 128 + pc * 32, half * 128 + pc * 32 + 32)
                                        ocols = slice(bk * 512 + r, bk * 512 + r + 31 * 16 + 1, 16)
                                    elif d == 4:
                                        bk = cb // 4
                                        qs = slice(half * 128, half * 128 + 128)
                                        ocols = slice(bk * 512 + r, bk * 512 + r + 127 * 4 + 1, 4)
                                    else:
                                        bk = cb // 4
                                        qs = slice(half * 128, half * 128 + 128)
                                        lo = (cb % 4) * 128
                                        ocols = slice(bk * 512 + lo, bk * 512 + lo + 128)
                                    st_ = first_in_bank[bk]
                                    first_in_bank[bk] = False
                                    P.op("pe", lambda e, vc=vc, qs=qs, ocols=ocols, st_=st_: e.matmul(
                                        psum[0:128, ocols], vp[hb][p][:, vc, :], wsb[si][:, qs],
                                        start=st_, stop=True, skip_group_check=True),
                                        reads=[("dvp", 0, p, (vc // d if d > 1 else 0)), ("dvp1", p), ("dws", si)],
                                        writes=[PS(bk)])

                        LOOK = 2
                        slots = []
                        for ci, cmb in enumerate(combos):
                            slots.append((nsc % 4, 4 + nsc % 4))
                            nsc += 1
                        for ci in range(len(combos) + LOOK):
                            if ci < len(combos):
                                stage_a(*combos[ci], *slots[ci])
                            if ci >= LOOK:
                                stage_b(*combos[ci - LOOK], *slots[ci - LOOK])
                        for bk in range(4):
                            if bk % 2 == 0:
                                P.op("act", lambda e, bk=bk: e.activation(
                                    accs[:, bk * 512:(bk + 1) * 512], bank(bk), AF.Copy),
                                    reads=[PS(bk)], writes=[("daccs", bk)])
                            else:
                                P.op("dve", lambda e, bk=bk: e.tensor_copy(
                                    accs[:, bk * 512:(bk + 1) * 512], bank(bk)),
                                    reads=[PS(bk)], writes=[("daccs", bk)])
                        for bk in range(4):
                            pb = 4 + nsc % 4
                            nsc += 1
                            i2 = bk % 2
                            P.op("pe", lambda e, bk=bk, pb=pb: e.matmul(
                                bank(pb, 0, 512, 0, 64), shiftm[:, :], accs[:, bk * 512:(bk + 1) * 512],
                                start=True, stop=True),
                                reads=["shiftm", ("daccs", bk)], writes=[PS(pb)])
                            P.op("act", lambda e, pb=pb, i2=i2: e.activation(
                                lnd[i2][:], bank(pb, 0, 512, 0, 64), AF.Ln),
                                reads=[PS(pb)], writes=[("dlnd", i2)])
                            P.op("act", lambda e, i2=i2: e.activation(rdn[i2][:], lnd[i2][:], AF.Exp, scale=-1.0),
                                 reads=[("dlnd", i2)], writes=[("drdn", i2)])
                            P.op("dve", lambda e, bk=bk, i2=i2: e.tensor_tensor(
                                ost[i2][:], accs[0:64, bk * 512:(bk + 1) * 512], rdn[i2][:], ALU.mult),
                                reads=[("daccs", bk), ("drdn", i2)], writes=[("dost", i2)])
                            c0 = sbk * 2048 + bk * 512
                            P.dma("sp", attT[h * 64:(h + 1) * 64, c0:c0 + 512], ost[i2][:],
                                  reads=[("dost", i2)], writes=[("attT", h, c0 // 512)])
                P.barrier()

        if 2 in phases:
            phase_2()

        def phase_3():
            with contextlib.ExitStack() as ps3:
                qp = [sb(f"sqp{i}", [128, S], BF16, ps3) for i in range(2)]
                kp = [sb(f"skp{i}", [128, S], BF16, ps3) for i in range(2)]
                vp = [sb(f"svp{i}", [128, NT, 128], BF16, ps3) for i in range(2)]
                eb = [sb(f"seb{i}", [128, 1024], F32, ps3) for i in range(3)]
                spb = [sb(f"ssp{i}", [128, 1024], BF16, ps3) for i in range(2)]
                ub = [sb(f"sub{i}", [128, 1024], F32, ps3) for i in range(2)]
                wb = [sb(f"swb{i}", [128, 1024], BF16, ps3) for i in range(2)]
                osb = [sb(f"sos{i}", [128, 512], F32, ps3) for i in range(2)]
                OB = 6

                def sb_pair(hp, pb_):
                    row = 512 + hp * 128
                    P.dma("sp", qp[pb_][:], qT[row:row + 128, :],
                          reads=[("qk", id(qT), row, g) for g in range(NG)], writes=[("sqp", pb_)])
                    P.dma("sp", kp[pb_][:], kT[row:row + 128, :],
                          reads=[("qk", id(kT), row, g) for g in range(NG)], writes=[("skp", pb_)])
                    P.dma("sp", vp[pb_][:], vv[:, row:row + 128].rearrange("(t p) e -> p t e", p=128),
                          reads=[("vv", t) for t in range(NT)], writes=[("svp", pb_)])
                    rdqk = [("sqp", pb_), ("skp", pb_)]

                    def emit_z(g, kb, i):
                        v = kb - 4 * g
                        diag = v >= 0
                        qs = slice(g * 512, (g + 1) * 512)
                        ks = slice(kb * 128, (kb + 1) * 128)
                        for s_ in range(2):
                            zb = 2 * (i % 2) + s_
                            ps_ = slice(64 * s_, 64 * s_ + 64)
                            P.op("pe", lambda e, zb=zb, ps_=ps_: e.matmul(
                                bank(zb), kp[pb_][ps_, ks], qp[pb_][ps_, qs], start=True, stop=not diag),
                                reads=rdqk, writes=[PS(zb)])
                        if diag:
                            for s_ in range(2):
                                zb = 2 * (i % 2) + s_
                                P.op("pe", lambda e, zb=zb: e.matmul(
                                    bank(zb), identb[:], mbb[:, v * 512:(v + 1) * 512], start=False, stop=True),
                                    reads=["identb", "mbb"], writes=[PS(zb)])

                    hs2 = lambda s_: slice(512 * s_, 512 * s_ + 512)

                    def emit_e(i):
                        j, j3 = i % 2, i % 3
                        P.op("act", lambda e: e.activation(eb[j3][:], psum[:, 2 * j * 512:(2 * j + 2) * 512], AF.Exp),
                             reads=[PS(2 * j), PS(2 * j + 1)], writes=[("seb", j3)])

                    def emit_sp(i):
                        j, j3 = i % 2, i % 3
                        P.op("act", lambda e: e.activation(spb[j][:], eb[j3][:], AF.Ln, bias=1.0),
                             reads=[("seb", j3)], writes=[("ssp", j)])

                    def emit_negM(i):
                        j = i % 2
                        for s_ in range(2):
                            P.op("pe", lambda e, s_=s_: e.matmul(bank(4 + s_), negMb[:], spb[j][:, hs2(s_)],
                                                                 start=(i == 0), stop=True, skip_group_check=True),
                                 reads=["negMb", ("ssp", j)], writes=[PS(4 + s_)])

                    def emit_u(i):
                        j = i % 2
                        P.op("act", lambda e: e.activation(ub[j][:], psum[:, 4 * 512:6 * 512], AF.Exp),
                             reads=[PS(4), PS(5)], writes=[("sub", j)])

                    def emit_w(i):
                        j, j3 = i % 2, i % 3
                        P.op("dve", lambda e: e.tensor_tensor(wb[j][:], eb[j3][:], ub[j][:], ALU.mult),
                             reads=[("seb", j3), ("sub", j)], writes=[("swb", j)])

                    def emit_negN(i):
                        j = i % 2
                        for s_ in range(2):
                            P.op("pe", lambda e, s_=s_: e.matmul(bank(4 + s_), negNb[:], spb[j][:, hs2(s_)],
                                                                 start=False, stop=True, skip_group_check=True),
                                 reads=["negNb", ("ssp", j), ("sub", j)], writes=[PS(4 + s_)])

                    def emit_pv(kb, i, n):
                        j = i % 2
                        for s_ in range(2):
                            P.op("pe", lambda e, s_=s_: e.matmul(
                                bank(OB, 0, 512, 64 * s_, 64 * s_ + 64), vp[pb_][:, kb, 64 * s_:64 * s_ + 64],
                                wb[j][:, hs2(s_)], start=(i == 0), stop=(i == n - 1), skip_group_check=True),
                                reads=[("svp", pb_), ("swb", j)], writes=[PS(OB)])

                    for g in range(NG):
                        kbs = list(range(4 * g + 3, -1, -1))
                        n = len(kbs)
                        emit_z(g, kbs[0], 0)
                        emit_z(g, kbs[1], 1)
                        emit_e(0)
                        emit_z(g, kbs[2], 2)
                        emit_e(1)
                        emit_sp(0)
                        emit_negM(0)
                        for i in range(n):
                            if i + 1 < n:
                                emit_sp(i + 1)
                            emit_u(i)
                            if i + 2 < n:
                                emit_e(i + 2)
                            emit_w(i)
                            if i + 1 < n:
                                emit_negN(i)
                                emit_negM(i + 1)
                            emit_pv(kbs[i], i, n)
                            if i + 3 < n:
                                emit_z(g, kbs[i + 3], i + 3)
                        ob_ = g % 2
                        P.op("dve", lambda e, ob_=ob_: e.tensor_copy(osb[ob_][:], bank(OB)),
                             reads=[PS(OB)], writes=[("sos", ob_)])
                        P.dma("sp", attT[row:row + 128, g * 512:(g + 1) * 512], osb[ob_][:],
                              reads=[("sos", ob_)], writes=[("attT", 8 + 2 * hp, g), ("attT", 9 + 2 * hp, g)])

                for hp in range(NH // 2):
                    sb_pair(hp, hp % 2)
                P.barrier()

        if 3 in phases:
            phase_3()

        def phase_4():
            with contextlib.ExitStack() as ps4:
                wout = sb("wout", [128, 8, D], BF16, ps4)
                wrt = sb("wrt", [128, 8, 36], F32, ps4)
                at = [sb(f"at{i}", [128, 8, 512], F32, ps4) for i in range(2)]
                sq = [sb(f"sq{i}", [128, 512], F32, ps4) for i in range(2)]
                lnt = [sb(f"lnt{i}", [128, 512], F32, ps4) for i in range(2)]
                rsb = [sb(f"rsb{i}", [128, 512], F32, ps4) for i in range(2)]
                mixT = [sb(f"mixT{i}", [128, 8, 512], BF16, ps4) for i in range(2)]
                xb = [sb(f"exb{i}", [128, D], F32, ps4) for i in range(2)]
                x1b = [sb(f"ex1b{i}", [128, D], F32, ps4) for i in range(2)]
                yb = [sb(f"eyb{i}", [128, D], F32, ps4) for i in range(2)]
                junk = sb("junk4", [128, D], BF16, ps4)
                st = [[sb(f"est{n}{i}", [128, 1], F32, ps4) for i in range(2)] for n in ("ss", "ln", "rs")]
                h2b = [sb(f"h2b{i}", [128, 8, 512], BF16, ps4) for i in range(2)]
                h2f = [sb(f"h2f{i}", [128, 8, 128], F32, ps4) for i in range(2)]
                rt = {n: [sb(f"rt_{n}{i}", [128, w_], F32, ps4) for i in range(2)]
                      for n, w_ in (("lg", 36), ("gm", 1), ("gs", 4), ("ge", 4), ("gsum", 1), ("gg", 1), ("oh", 4),
                                    ("sel", 8), ("t8", 8), ("d21", 1), ("ed", 1), ("den", 1), ("w1", 1), ("w2", 1),
                                    ("m1", 8), ("m2", 8), ("wl", 8))}
                for k in range(8):
                    load_cast(wout[:, k, :], w_out[k * 128:(k + 1) * 128, :], 1024, ("pool", "dve")[k % 2], [("wout", k)])
                P.dma("sp", wrt[:], w_rt.rearrange("(k p) n -> p k n", p=128), writes=["wrt"])
                att_all = lambda g: [("attT", h, g) for h in range(16)]
                for g in range(NG):
                    gb_ = g % 2
                    P.dma("sp", at[gb_][:], attT[:, g * 512:(g + 1) * 512].rearrange("(c p) s -> p c s", p=128),
                          reads=att_all(g), writes=[("at", gb_)])
                    for grp in range(2):
                        pb = 6 + grp
                        for c4 in range(4):
                            c = grp * 4 + c4
                            si = c % 2
                            P.op("act", lambda e, c=c, si=si, gb_=gb_: e.activation(sq[si][:], at[gb_][:, c, :], AF.Square),
                                 reads=[("at", gb_)], writes=[("sq", si)])
                            P.op("pe", lambda e, si=si, pb=pb, c4=c4: e.matmul(
                                bank(pb), ones_f[:, :], sq[si][:], start=(c4 == 0), stop=(c4 == 3)),
                                reads=["ones_f", ("sq", si)], writes=[PS(pb)])
                        P.op("act", lambda e, pb=pb, grp=grp: e.activation(
                            lnt[grp][:], bank(pb), AF.Ln, bias=EPS, scale=1.0 / 512),
                            reads=[PS(pb)], writes=[("lnt", grp)])
                        P.op("act", lambda e, grp=grp: e.activation(rsb[grp][:], lnt[grp][:], AF.Exp, scale=-0.5),
                             reads=[("lnt", grp)], writes=[("rsb", grp)])
                        for c4 in range(4):
                            c = grp * 4 + c4
                            P.op("dve", lambda e, c=c, gb_=gb_, grp=grp: e.scalar_tensor_tensor(
                                mixT[gb_][:, c, :], at[gb_][:, c, :], vecT[:, 24 + c:25 + c], rsb[grp][:],
                                ALU.mult, ALU.mult),
                                reads=[("at", gb_), "vecT", ("rsb", grp)], writes=[("mixT", gb_, c)])
                    mix_all = [("mixT", gb_, c) for c in range(8)]

                    def s1(tt, g=g, gb_=gb_, mix_all=mix_all):
                        t = g * 4 + tt
                        b = t % 2
                        P.dma("sp", xb[b][:], x[t * 128:(t + 1) * 128, :], writes=[("exb", b)])
                        for half in range(2):
                            pb = 4 + half
                            for k in range(8):
                                P.op("pe", lambda e, k=k, half=half, pb=pb: e.matmul(
                                    bank(pb), mixT[gb_][:, k, tt * 128:(tt + 1) * 128],
                                    wout[:, k, half * 512:(half + 1) * 512], start=(k == 0), stop=(k == 7)),
                                    reads=mix_all + [("wout", k_) for k_ in range(8)], writes=[PS(pb)])
                            P.op("dve", lambda e, half=half, pb=pb: e.tensor_tensor(
                                x1b[b][:, half * 512:(half + 1) * 512], bank(pb),
                                gmix_bc[:, half * 512:(half + 1) * 512], ALU.mult),
                                reads=[PS(pb), ("gmix_bc", half)], writes=[("ex1b", b, half)])
                            P.op("pool", lambda e, half=half: e.tensor_tensor(
                                x1b[b][:, half * 512:(half + 1) * 512], x1b[b][:, half * 512:(half + 1) * 512],
                                xb[b][:, half * 512:(half + 1) * 512], ALU.add),
                                reads=[("ex1b", b, half), ("exb", b)], writes=[("ex1b", b, half)])
                        x1tok = [("ex1b", b, 0), ("ex1b", b, 1)]
                        P.dma("sp", x1[t * 128:(t + 1) * 128, :], x1b[b][:], reads=x1tok, writes=[("x1", t)])

                    def s2a(tt, g=g):
                        t = g * 4 + tt
                        b = t % 2
                        x1tok = [("ex1b", b, 0), ("ex1b", b, 1)]
                        norm_stats(x1b[b], x1tok, ("n4", b), st[0][b], st[1][b], st[2][b], junk, yb[b], ("eyb", b))

                    def s2b(tt, g=g, gb_=gb_):
                        t = g * 4 + tt
                        b = t % 2
                        norm_transp(16,
                                    [lambda j: h2b[gb_][:, j, tt * 128:(tt + 1) * 128], lambda j: h2f[b][:, j, :]],
                                    [("h2b", gb_, tt), ("h2f", b)], (2 * b, 2 * b + 1), yb[b], ("eyb", b), act_only=True)

                    def s3(tt, g=g):
                        t = g * 4 + tt
                        b = t % 2
                        R = {n: v_[b] for n, v_ in rt.items()}
                        pb = 6 + (t % 2)
                        for k in range(8):
                            P.op("pe", lambda e, k=k: e.matmul(
                                bank(pb, 0, 36), h2f[b][:, k, :], wrt[:, k, :], start=(k == 0), stop=(k == 7)),
                                reads=[("h2f", b), "wrt"], writes=[PS(pb)])

                        def V(n):
                            return ("rt", n, b)

                        def dve(fn, reads, writes):
                            P.op("dve", fn, reads=reads, writes=writes)

                        dve(lambda e: e.tensor_copy(R["lg"][:], bank(pb, 0, 36)), [PS(pb)], [V("lg")])
                        dve(lambda e: e.tensor_reduce(R["gm"][:], R["lg"][:, 0:4], AX.X, ALU.max), [V("lg")], [V("gm")])
                        dve(lambda e: e.tensor_scalar(R["gs"][:], R["lg"][:, 0:4], R["gm"][:, 0:1], None, ALU.subtract),
                            [V("lg"), V("gm")], [V("gs")])
                        P.op("act", lambda e: e.activation(R["ge"][:], R["gs"][:], AF.Exp, accum_out=R["gsum"][:]),
                             reads=[V("gs")], writes=[V("ge"), V("gsum")])
                        dve(lambda e: e.reciprocal(R["gg"][:], R["gsum"][:]), [V("gsum")], [V("gg")])
                        dve(lambda e: e.tensor_scalar(R["oh"][:], R["lg"][:, 0:4], R["gm"][:, 0:1], None, ALU.is_equal),
                            [V("lg"), V("gm")], [V("oh")])
                        dve(lambda e: e.tensor_scalar(R["sel"][:], R["lg"][:, 4:12], R["oh"][:, 0:1], None, ALU.mult),
                            [V("lg"), V("oh")], [V("sel")])
                        for gi in range(1, 4):
                            dve(lambda e, gi=gi: e.scalar_tensor_tensor(
                                R["sel"][:], R["lg"][:, 4 + 8 * gi:12 + 8 * gi], R["oh"][:, gi:gi + 1], R["sel"][:],
                                ALU.mult, ALU.add), [V("lg"), V("oh"), V("sel")], [V("sel")])
                        dve(lambda e: e.max(R["t8"][:], R["sel"][:]), [V("sel")], [V("t8")])
                        dve(lambda e: e.tensor_tensor(R["d21"][:], R["t8"][:, 1:2], R["t8"][:, 0:1], ALU.subtract),
                            [V("t8")], [V("d21")])
                        P.op("act", lambda e: e.activation(R["ed"][:], R["d21"][:], AF.Exp),
                             reads=[V("d21")], writes=[V("ed")])
                        dve(lambda e: e.tensor_scalar(R["den"][:], R["ed"][:], 1.0, None, ALU.add), [V("ed")], [V("den")])
                        dve(lambda e: e.reciprocal(R["den"][:], R["den"][:]), [V("den")], [V("den")])
                        dve(lambda e: e.tensor_tensor(R["w1"][:], R["den"][:], R["gg"][:], ALU.mult),
                            [V("den"), V("gg")], [V("w1")])
                        dve(lambda e: e.tensor_tensor(R["w2"][:], R["w1"][:], R["ed"][:], ALU.mult),
                            [V("w1"), V("ed")], [V("w2")])
                        dve(lambda e: e.tensor_scalar(R["m1"][:], R["sel"][:], R["t8"][:, 0:1], R["w1"][:, 0:1],
                                                      ALU.is_equal, ALU.mult),
                            [V("sel"), V("t8"), V("w1")], [V("m1")])
                        dve(lambda e: e.tensor_scalar(R["m2"][:], R["sel"][:], R["t8"][:, 1:2], R["w2"][:, 0:1],
                                                      ALU.is_equal, ALU.mult),
                            [V("sel"), V("t8"), V("w2")], [V("m2")])
                        dve(lambda e: e.tensor_tensor(R["wl"][:], R["m1"][:], R["m2"][:], ALU.add),
                            [V("m1"), V("m2")], [V("wl")])
                        for gi in range(4):
                            dve(lambda e, gi=gi: e.tensor_scalar(
                                Wr[:, t * 32 + gi * 8:t * 32 + gi * 8 + 8], R["wl"][:], R["oh"][:, gi:gi + 1], None,
                                ALU.mult), [V("wl"), V("oh")], [("Wr", t)])

                    for t0_ in (0, 2):
                        s1(t0_)
                        s1(t0_ + 1)
                        s2a(t0_)
                        s2a(t0_ + 1)
                        s2b(t0_)
                        s2b(t0_ + 1)
                        s3(t0_)
                        s3(t0_ + 1)
                    P.dma("sp", h2T[:, g * 512:(g + 1) * 512].rearrange("(c p) s -> p c s", p=128), h2b[gb_][:],
                          reads=[("h2b", gb_, tt) for tt in range(4)], writes=[("h2T", g)])
                P.barrier()

        if 4 in phases:
            phase_4()

        def phase_5():
            with contextlib.ExitStack() as ps5:
                GT = 1024
                NGT = S // GT
                hg = sb("hg", [128, 8, GT], BF16, ps5)
                acc = [sb(f"acc{i}", [128, D], F32, ps5) for i in range(GT // 128)]
                wg = [sb(f"wg{i}", [128, 8, DFF], BF16, ps5) for i in range(2)]
                wu = [sb(f"wu{i}", [128, 8, DFF], BF16, ps5) for i in range(2)]
                wd = [sb(f"wd{i}", [128, 4, D], BF16, ps5) for i in range(2)]
                sgt = [sb(f"sgt{i}", [128, 512], F32, ps5) for i in range(2)]
                hm = [sb(f"hm{i}", [128, 4, 512], BF16, ps5) for i in range(2)]
                x1b = [sb(f"fx1b{i}", [128, D], F32, ps5) for i in range(2)]
                ob = [sb(f"fob{i}", [128, D], F32, ps5) for i in range(2)]
                junk = sb("junk5", [128, D], BF16, ps5)
                st = [[sb(f"fst{n}{i}", [128, 1], F32, ps5) for i in range(2)] for n in ("ss", "ln", "rs")]
                nld = 0
                npd = [0]
                if True:
                    def loads(ex, wbuf):
                        v3 = ("p (k f) -> p k f", dict(f=512))
                        for kh_ in range(2):
                            load_cast(wg[wbuf][:, kh_ * 4:(kh_ + 1) * 4, :],
                                      w_gate[ex][kh_ * 512:(kh_ + 1) * 512, :].rearrange("(k p) f -> p k f", p=128),
                                      2048, "act", [("wg", wbuf, kh_)], view=v3)
                            load_cast(wu[wbuf][:, kh_ * 4:(kh_ + 1) * 4, :],
                                      w_up[ex][kh_ * 512:(kh_ + 1) * 512, :].rearrange("(k p) f -> p k f", p=128),
                                      2048, ("act", "dve")[kh_], [("wu", wbuf, kh_)], view=v3)
                        for kh_ in range(2):
                            load_cast(wd[wbuf][:, :, kh_ * 512:(kh_ + 1) * 512],
                                      w_down[ex][:, kh_ * 512:(kh_ + 1) * 512].rearrange("(k p) f -> p k f", p=128),
                                      2048, "pool", [("wd", wbuf, kh_)], view=v3)

                    def gate_up(ex, sg, wbuf, hb):
                        for fc in range(4):
                            for (wt_, wn, pb) in ((wg, "wg", 0 + 2 * (fc % 2)), (wu, "wu", 1 + 2 * (fc % 2))):
                                for k in range(8):
                                    P.op("pe", lambda e, wt_=wt_, k=k, fc=fc, pb=pb: e.matmul(
                                        bank(pb), wt_[wbuf][:, k, fc * 128:(fc + 1) * 128],
                                        hg[:, k, sg * 512:(sg + 1) * 512], start=(k == 0), stop=(k == 7)),
                                        reads=[(wn, wbuf, 0), (wn, wbuf, 1), "hg"], writes=[PS(pb)])
                            pg = 0 + 2 * (fc % 2)
                            pu = 1 + 2 * (fc % 2)
                            si = fc % 2
                            P.op("act", lambda e, si=si, pg=pg: e.activation(sgt[si][:], bank(pg), AF.Silu),
                                 reads=[PS(pg)], writes=[("sgt", si)])
                            P.op("dve", lambda e, fc=fc, si=si, pu=pu: e.tensor_tensor(
                                hm[hb][:, fc, :], sgt[si][:], bank(pu), ALU.mult),
                                reads=[("sgt", si), PS(pu)], writes=[("hm", hb, fc)])

                    def down(ex, sg, wbuf, hb, G):
                        hm_all = [("hm", hb, fc) for fc in range(4)]
                        for tt in range(4):
                            tl = sg * 4 + tt
                            tg = G * (GT // 128) + tl
                            for half in range(2):
                                pb = 4 + npd[0] % 4
                                npd[0] += 1
                                for fc in range(4):
                                    P.op("pe", lambda e, fc=fc, tt=tt, half=half, pb=pb: e.matmul(
                                        bank(pb), hm[hb][:, fc, tt * 128:(tt + 1) * 128],
                                        wd[wbuf][:, fc, half * 512:(half + 1) * 512], start=(fc == 0), stop=(fc == 3)),
                                        reads=hm_all + [("wd", wbuf, half)], writes=[PS(pb)])
                                wcol = Wr[:, tg * 32 + ex:tg * 32 + ex + 1]
                                dst = acc[tl][:, half * 512:(half + 1) * 512]
                                if ex == 0:
                                    P.op("dve", lambda e, dst=dst, pb=pb, wcol=wcol: e.tensor_scalar(
                                        dst, bank(pb), wcol, None, ALU.mult),
                                        reads=[PS(pb), ("Wr", tg)], writes=[("acc", tl, half)])
                                else:
                                    P.op("dve", lambda e, dst=dst, pb=pb, wcol=wcol: e.scalar_tensor_tensor(
                                        dst, bank(pb), wcol, dst, ALU.mult, ALU.add),
                                        reads=[PS(pb), ("Wr", tg), ("acc", tl, half)], writes=[("acc", tl, half)])

                    def final_norm(G):
                        for tl in range(GT // 128):
                            tg = G * (GT // 128) + tl
                            b = tg % 2
                            P.dma("sp", x1b[b][:], x1[tg * 128:(tg + 1) * 128, :], reads=[("x1", tg)], writes=[("fx1b", b)])
                            for half in range(2):
                                hs_ = slice(half * 512, (half + 1) * 512)
                                fe = ("pool", "dve")[half]
                                P.op(fe, lambda e, tl=tl, hs_=hs_: e.tensor_tensor(
                                    acc[tl][:, hs_], acc[tl][:, hs_], gffn_bc[:, hs_], ALU.mult),
                                    reads=[("acc", tl, half), ("gffn_bc", half)], writes=[("acc", tl, half)])
                                P.op(fe, lambda e, tl=tl, hs_=hs_, b=b: e.tensor_tensor(
                                    acc[tl][:, hs_], acc[tl][:, hs_], x1b[b][:, hs_], ALU.add),
                                    reads=[("acc", tl, half), ("fx1b", b)], writes=[("acc", tl, half)])
                            a_all = [("acc", tl, 0), ("acc", tl, 1)]
                            P.op("act", lambda e, tl=tl, b=b: e.activation(junk[:], acc[tl][:], AF.Square, accum_out=st[0][b][:]),
                                 reads=a_all, writes=[("fjunk",), ("fss", b)])
                            P.op("act", lambda e, b=b: e.activation(st[1][b][:], st[0][b][:], AF.Ln, bias=EPS, scale=1.0 / D),
                                 reads=[("fss", b)], writes=[("fln", b)])
                            P.op("act", lambda e, b=b: e.activation(st[2][b][:], st[1][b][:], AF.Exp, scale=-0.5),
                                 reads=[("fln", b)], writes=[("frs", b)])
                            P.op("dve", lambda e, tl=tl, b=b: e.scalar_tensor_tensor(
                                ob[b][:], acc[tl][:], st[2][b][:, 0:1], gfin_bc[:], ALU.mult, ALU.mult),
                                reads=a_all + [("frs", b), ("gfin_bc", 0), ("gfin_bc", 1)], writes=[("fob", b)])
                            P.dma("sp", out[tg * 128:(tg + 1) * 128, :], ob[b][:], reads=[("fob", b)], writes=[("out", tg)])

                    pending = None
                    nu = 0
                    for G in range(NGT):
                        for ex in range(NEXP):
                            wbuf = nld % 2
                            nld += 1
                            if ex == 0:
                                P.dma("sp", hg[:], h2T[:, G * GT:(G + 1) * GT].rearrange("(c p) s -> p c s", p=128),
                                      reads=[("h2T", G * 4 + i) for i in range(4)], writes=["hg"])
                            loads(ex, wbuf)
                            for sg in range(GT // 512):
                                gate_up(ex, sg, wbuf, nu % 2)
                                if pending is not None:
                                    down(*pending)
                                    if pending[4] != G:
                                        final_norm(pending[4])
                                pending = (ex, sg, wbuf, nu % 2, G)
                                nu += 1
                    down(*pending)
                    final_norm(pending[4])
                P.barrier()

        if 5 in phases:
            phase_5()

        P.finish()
    return nc


_CACHE = {}


def _get_nc(S, phases=(0, 1, 2, 3, 4, 5), dbg=False):
    key = (S, tuple(phases), dbg)
    if key not in _CACHE:
        _CACHE[key] = build_nc(S, phases, dbg)
    return _CACHE[key]


def make_in_maps(inputs, S, n):
    f = np.float32
    consts = _host_consts()
    shared = {
        "w_ada": np.ascontiguousarray(inputs["w_ada"][0], f),
        "b_ada": np.ascontiguousarray(inputs["b_ada"][0:1], f),
        "g_mix": np.ascontiguousarray(inputs["g_mix"][0:1], f),
        "w_in": np.ascontiguousarray(inputs["w_in"][0], f),
        "g_dil_out": np.ascontiguousarray(inputs["g_dil_out"][0:1], f),
        "g_sb_out": np.ascontiguousarray(inputs["g_sb_out"][0:1], f),
        "w_out": np.ascontiguousarray(inputs["w_out"][0], f),
        "g_ffn": np.ascontiguousarray(inputs["g_ffn"][0:1], f),
        "w_rt": np.ascontiguousarray(np.concatenate([inputs["w_group"][0], inputs["w_expert"][0]], axis=1), f),
        "w_gate": np.ascontiguousarray(inputs["w_gate"][0], f),
        "w_up": np.ascontiguousarray(inputs["w_up"][0], f),
        "w_down": np.ascontiguousarray(inputs["w_down"][0], f),
        "g_final": np.ascontiguousarray(np.asarray(inputs["g_final"]).reshape(1, D), f),
    }
    for k, v in consts.items():
        shared["k_" + k] = v
    maps = []
    for b in range(n):
        m = dict(shared)
        m["x"] = np.ascontiguousarray(inputs["x"][b, :S], f)
        m["c"] = np.ascontiguousarray(inputs["c"][b:b + 1], f)
        maps.append(m)
    return maps


def kernel(**inputs):
    inputs = {k: np.asarray(v) for k, v in inputs.items()}
    B, S, _ = inputs["x"].shape
    nc = _get_nc(S)
    maps = make_in_maps(inputs, S, B)
    res = run_bass_kernel_spmd(nc, maps, core_ids=list(range(B)))
    return np.stack([np.asarray(r["out"], dtype=np.float32) for r in res.results], axis=0)
```
